# Optimizing a Trainium2 kernel written in Bass

```python
import jax, jax.numpy as jnp
from jax import lax
import numpy as np

D_MODEL = 2048
BATCH = 4
SEQ = 2048
DEPTH = 1
DEC_BATCH = 128
DEC_SEQ = 8
PAST_LEN = 16384
PAGE_SIZE = 128

MIX_WIDTH = D_MODEL
RET_WIDTH = MIX_WIDTH // 2
CONV_WIDTH = MIX_WIDTH - RET_WIDTH
N_RET_HEADS = 8
RET_HEAD_DIM = RET_WIDTH // N_RET_HEADS
RET_CHUNK = 128
ROPE_BASE = 10000.0
CONV_K = 3
N_EXPERTS = 32
TOP_K = 4
D_FF = D_MODEL
SWIGLU_LIMIT = 7.0
SWIGLU_ALPHA = 1.702
NORM_EPS = 1e-6
N_MOD = 6
IN_COLS = 4 * RET_WIDTH + 3 * CONV_WIDTH
IN_SPLITS = (RET_WIDTH, 2 * RET_WIDTH, 3 * RET_WIDTH, 4 * RET_WIDTH,
             4 * RET_WIDTH + CONV_WIDTH, 4 * RET_WIDTH + 2 * CONV_WIDTH)

kernel_name = 'hybrid_retention_shortconv_moe_adaln_step'


def rms_norm(x, g):
    xf = x.astype(jnp.float32)
    y = xf * lax.rsqrt(jnp.mean(xf * xf, axis=-1, keepdims=True) + NORM_EPS)
    return (y * g.astype(jnp.float32)).astype(x.dtype)


def rope(x, pos):
    half = x.shape[-1] // 2
    inv_freq = ROPE_BASE ** (-jnp.arange(half, dtype=jnp.float32) / half)
    ang = pos[:, None] * inv_freq[None, :]
    cos = jnp.cos(ang)[None, :, None, :]
    sin = jnp.sin(ang)[None, :, None, :]
    x1, x2 = x[..., :half], x[..., half:]
    return jnp.concatenate([x1 * cos - x2 * sin, x1 * sin + x2 * cos], axis=-1)


def retention_log_decay():
    return jnp.log1p(-jnp.exp2(-5.0 - jnp.arange(N_RET_HEADS, dtype=jnp.float32)))


def retention_chunk(S, qkv, log_g):
    q, k, v = qkv
    L = q.shape[1]
    idx = jnp.arange(L, dtype=jnp.float32)
    rel = idx[:, None] - idx[None, :]
    causal = rel >= 0
    decay = jnp.where(causal[None], jnp.exp(log_g[:, None, None] * jnp.maximum(rel, 0.0)[None]), 0.0)
    scores = jnp.einsum('bihd,bjhd->bhij', q, k) * decay[None]
    inner = jnp.einsum('bhij,bjhe->bihe', scores, v)
    q_decay = jnp.exp(log_g[None, :] * (idx[:, None] + 1.0))
    cross = jnp.einsum('bihd,bhde->bihe', q * q_decay[None, :, :, None], S)
    k_decay = jnp.exp(log_g[None, :] * (L - 1.0 - idx)[:, None])
    S_new = jnp.exp(log_g * L)[None, :, None, None] * S + jnp.einsum('bjhd,bjhe->bhde', k * k_decay[None, :, :, None], v)
    return S_new, inner + cross


def retention(q, k, v, S0, log_g):
    B, T, H, d = q.shape
    L = RET_CHUNK if T % RET_CHUNK == 0 else T
    nc = T // L

    def to_chunks(a):
        return a.reshape(B, nc, L, H, a.shape[-1]).swapaxes(0, 1)

    S, o = lax.scan(lambda s, c: retention_chunk(s, c, log_g), S0, (to_chunks(q), to_chunks(k), to_chunks(v)))
    return o.swapaxes(0, 1).reshape(B, T, H, -1), S


def mixer(h, pos, s_ret, s_conv, w_in, conv_w, ret_gn, w_o):
    B, T, _ = h.shape
    f32 = jnp.float32
    proj = h @ w_in
    q, k, v, g, gb, gc, u = jnp.split(proj, IN_SPLITS, axis=-1)
    q = rope(q.astype(f32).reshape(B, T, N_RET_HEADS, RET_HEAD_DIM), pos)
    k = rope(k.astype(f32).reshape(B, T, N_RET_HEADS, RET_HEAD_DIM), pos) * (RET_HEAD_DIM ** -0.5)
    v = v.astype(f32).reshape(B, T, N_RET_HEADS, RET_HEAD_DIM)
    o, S = retention(q, k, v, s_ret.astype(f32), retention_log_decay())
    mu = jnp.mean(o, axis=-1, keepdims=True)
    var = jnp.mean(jnp.square(o - mu), axis=-1, keepdims=True)
    o = ((o - mu) * lax.rsqrt(var + NORM_EPS)).reshape(B, T, RET_WIDTH) * ret_gn.astype(f32)
    ret_out = (jax.nn.silu(g.astype(f32)) * o).astype(h.dtype)
    z = gc * u
    zp = jnp.concatenate([s_conv.astype(z.dtype), z], axis=1)
    conv = sum(conv_w[i] * zp[:, i:i + T] for i in range(CONV_K))
    conv_out = gb * conv
    y = jnp.concatenate([ret_out, conv_out], axis=-1) @ w_o
    return y, S.astype(s_ret.dtype), zp[:, -(CONV_K - 1):].astype(s_conv.dtype)


def moe(h, w_router, b_router, w_gu, b_gu, w_dn, b_dn):
    B, T, D = h.shape
    xt = h.reshape(B * T, D)
    logits = xt.astype(jnp.float32) @ w_router.astype(jnp.float32) + b_router.astype(jnp.float32)
    top_v, top_i = lax.top_k(logits, TOP_K)
    top_w = jax.nn.softmax(top_v, axis=-1)
    gates = jnp.einsum('nk,nke->ne', top_w, jax.nn.one_hot(top_i, N_EXPERTS, dtype=jnp.float32))

    def expert(acc, ws):
        wgu, bgu, wdn, bdn, gt = ws
        gu = xt @ wgu + bgu
        gate = jnp.minimum(gu[:, :D_FF], SWIGLU_LIMIT)
        up = jnp.clip(gu[:, D_FF:], -SWIGLU_LIMIT, SWIGLU_LIMIT)
        act = gate * jax.nn.sigmoid(SWIGLU_ALPHA * gate) * (up + 1.0)
        out = act @ wdn + bdn
        return acc + gt[:, None] * out.astype(jnp.float32), None

    acc, _ = lax.scan(expert, jnp.zeros((B * T, D), jnp.float32), (w_gu, b_gu, w_dn, b_dn, gates.T))
    return acc.astype(h.dtype).reshape(B, T, D)


def layer(x, c, pos, s_ret, s_conv, w_ada, b_ada, norm1_g, norm2_g, w_in, conv_w, ret_gn, w_o,
          w_router, b_router, w_gu, b_gu, w_dn, b_dn):
    B, T, D = x.shape
    mod = (jax.nn.silu(c) @ w_ada + b_ada).reshape(B, N_MOD, 1, D)
    shift1, scale1, gate1, shift2, scale2, gate2 = (mod[:, i] for i in range(N_MOD))
    h = rms_norm(x, norm1_g) * (1.0 + scale1) + shift1
    mix, s_ret_new, s_conv_new = mixer(h, pos, s_ret, s_conv, w_in, conv_w, ret_gn, w_o)
    x = x + gate1 * mix
    h2 = rms_norm(x, norm2_g) * (1.0 + scale2) + shift2
    x = x + gate2 * moe(h2, w_router, b_router, w_gu, b_gu, w_dn, b_dn)
    return x, s_ret_new, s_conv_new


def trunk(x, c, pos, ret0, conv0, w_ada, b_ada, norm1_g, norm2_g, w_in, conv_w, ret_gn, w_o,
          w_router, b_router, w_gu, b_gu, w_dn, b_dn, final_g):
    ret_new, conv_new = [], []
    for l in range(DEPTH):
        x, s, cv = layer(x, c, pos, ret0[l], conv0[l], w_ada[l], b_ada[l], norm1_g[l], norm2_g[l],
                         w_in[l], conv_w[l], ret_gn[l], w_o[l], w_router[l], b_router[l],
                         w_gu[l], b_gu[l], w_dn[l], b_dn[l])
        ret_new.append(s)
        conv_new.append(cv)
    return rms_norm(x, final_g), jnp.stack(ret_new), jnp.stack(conv_new)


def setup_inputs(seed: int = 0) -> dict:
    key = jax.random.key(seed)
    ks = jax.random.split(key, 24)
    f32 = jnp.float32
    nrm = lambda k, shape, s: jax.random.normal(k, shape, f32) * s
    return {
        'x_prompt': nrm(ks[0], (BATCH, SEQ, D_MODEL), 1.0),
        'x_sample': nrm(ks[1], (DEC_BATCH, DEC_SEQ, D_MODEL), 1.0),
        'state_ret': nrm(ks[2], (DEPTH, DEC_BATCH, N_RET_HEADS, RET_HEAD_DIM, RET_HEAD_DIM), 0.1),
        'state_conv': nrm(ks[3], (DEPTH, DEC_BATCH, CONV_K - 1, CONV_WIDTH), 1.0),
        'c_prompt': nrm(ks[4], (BATCH, D_MODEL), 1.0),
        'c_sample': nrm(ks[5], (DEC_BATCH, D_MODEL), 1.0),
        'w_ada': nrm(ks[6], (DEPTH, D_MODEL, N_MOD * D_MODEL), 0.5 * D_MODEL ** -0.5),
        'b_ada': nrm(ks[7], (DEPTH, N_MOD * D_MODEL), 0.02),
        'norm1_g': 1.0 + nrm(ks[8], (DEPTH, D_MODEL), 0.02),
        'norm2_g': 1.0 + nrm(ks[9], (DEPTH, D_MODEL), 0.02),
        'w_in': nrm(ks[10], (DEPTH, D_MODEL, IN_COLS), D_MODEL ** -0.5),
        'conv_w': nrm(ks[11], (DEPTH, CONV_K, CONV_WIDTH), CONV_K ** -0.5),
        'ret_gn': 1.0 + nrm(ks[12], (DEPTH, RET_WIDTH), 0.02),
        'w_o': nrm(ks[13], (DEPTH, MIX_WIDTH, D_MODEL), MIX_WIDTH ** -0.5),
        'w_router': nrm(ks[14], (DEPTH, D_MODEL, N_EXPERTS), D_MODEL ** -0.5),
        'b_router': nrm(ks[15], (DEPTH, N_EXPERTS), 0.01),
        'w_gu': nrm(ks[16], (DEPTH, N_EXPERTS, D_MODEL, 2 * D_FF), D_MODEL ** -0.5),
        'b_gu': nrm(ks[17], (DEPTH, N_EXPERTS, 2 * D_FF), 0.01),
        'w_dn': nrm(ks[18], (DEPTH, N_EXPERTS, D_FF, D_MODEL), D_FF ** -0.5),
        'b_dn': nrm(ks[19], (DEPTH, N_EXPERTS, D_MODEL), 0.01),
        'final_g': 1.0 + nrm(ks[20], (D_MODEL,), 0.02),
    }


def reference(x_prompt, x_sample, state_ret, state_conv, c_prompt, c_sample, w_ada, b_ada,
              norm1_g, norm2_g, w_in, conv_w, ret_gn, w_o, w_router, b_router, w_gu, b_gu,
              w_dn, b_dn, final_g):
    weights = (w_ada, b_ada, norm1_g, norm2_g, w_in, conv_w, ret_gn, w_o,
               w_router, b_router, w_gu, b_gu, w_dn, b_dn, final_g)
    bp, tp = x_prompt.shape[0], x_prompt.shape[1]
    pos_p = jnp.arange(tp, dtype=jnp.float32)
    ret0_p = jnp.zeros((DEPTH, bp) + state_ret.shape[2:], state_ret.dtype)
    conv0_p = jnp.zeros((DEPTH, bp) + state_conv.shape[2:], state_conv.dtype)
    y_prompt, ret_p, conv_p = trunk(x_prompt, c_prompt, pos_p, ret0_p, conv0_p, *weights)
    pos_s = PAST_LEN + jnp.arange(x_sample.shape[1], dtype=jnp.float32)
    y_sample, ret_s, conv_s = trunk(x_sample, c_sample, pos_s, state_ret, state_conv, *weights)
    return (y_prompt, y_sample, ret_p, conv_p, ret_s, conv_s)
```

```python
from contextlib import ExitStack
import numpy as np
import concourse.bass as bass
import concourse.mybir as mybir
from concourse.bass_utils import run_bass_kernel_spmd

F32 = mybir.dt.float32
F32R = mybir.dt.float32r
I32 = mybir.dt.int32
ALU = mybir.AluOpType
AF = mybir.ActivationFunctionType
AX = mybir.AxisListType

NCORES = 8
DEC_SEQ = 8
SEQ_PER_CORE = 16
ROPE_BASE = 10000.0
NORM_EPS = 1e-6
PAST_LEN = 16384
TOPK = 4
NRING = 24


NS_DEFAULT = 5


class Cfg:
    def __init__(s, D=2048, E=32, TP=8):
        s.D = D
        s.KD = D // 128
        s.RW = D // 2
        s.CW = D // 2
        s.H = s.RW // 128
        s.IN = 4 * s.RW + 3 * s.CW
        s.E = E
        s.F = D
        s.TP = TP
        s.NPRE = TP
        s.NT = TP + 1
        s.CB = 256
        s.CAP = 128 * NS_DEFAULT
        s.NS = s.CAP // 128
        s.GW = 256
        s.KH = max(1, s.KD // 2)
        s.NTOK = s.NT * 128


class SyncState:
    def __init__(s, nc, es):
        s.engs = ('pe', 'act', 'dve', 'pool', 'sp')
        s.sem = {e: es.enter_context(nc.semaphore("sem_" + e)) for e in s.engs}
        s.ring = [es.enter_context(nc.semaphore("ring%d" % i)) for i in range(NRING)]
        s.count = {e: 0 for e in s.engs}
        s.ringval = [0] * NRING
        s.ndma = 0


class Prog:
    def __init__(s, nc, st):
        s.nc = nc
        s.st = st
        s.ops = []

    def add(s, eng, fn, r=(), w=(), dma=False):
        s.ops.append((eng, fn, tuple(r), tuple(w), dma))

    def emit(s, final=False):
        nc, st, ops = s.nc, s.st, s.ops
        n = len(ops)
        deps = [None] * n
        last_w, readers = {}, {}
        for i, (eng, fn, r, w, dma) in enumerate(ops):
            d = set()
            for k in r:
                if k in last_w:
                    d.add(last_w[k])
            for k in w:
                if k in last_w:
                    d.add(last_w[k])
                d.update(readers.get(k, ()))
            d.discard(i)
            deps[i] = d
            for k in r:
                readers.setdefault(k, []).append(i)
            for k in w:
                last_w[k] = i
                readers[k] = []
        signal = [False] * n
        for i in range(n):
            ei = ops[i][0]
            for d in deps[i]:
                if ops[d][4]:
                    continue
                if ops[d][0] == ei and ei == 'pe':
                    continue
                signal[d] = True
        lastop = {}
        for i in range(n):
            if not ops[i][4]:
                lastop[ops[i][0]] = i
        for e, i in lastop.items():
            signal[i] = True
        start_count = dict(st.count)
        start_ring = list(st.ringval)
        tok = [None] * n
        for i in range(n):
            eng, fn, r, w, dma = ops[i]
            if dma:
                g = st.ndma
                st.ndma += 1
                ri = g % NRING
                st.ringval[ri] += 16
                tok[i] = (('ring', ri), st.ringval[ri])
            elif signal[i]:
                st.count[eng] += 1
                tok[i] = (('eng', eng), st.count[eng])
        end_count = dict(st.count)
        end_ring = list(st.ringval)

        def semof(key):
            return st.sem[key[1]] if key[0] == 'eng' else st.ring[key[1]]

        def run_engine(me, eobj):
            waited = {}

            def wait(key, val):
                if val <= 0 or waited.get(key, 0) >= val:
                    return
                eobj.wait_ge(semof(key), val)
                waited[key] = val
            for e in st.engs:
                wait(('eng', e), start_count[e])
            for ri in range(NRING):
                wait(('ring', ri), start_ring[ri])
            for i in range(n):
                eng, fn, r, w, dma = ops[i]
                if eng != me:
                    continue
                for d in sorted(deps[i]):
                    if ops[d][0] == me and me == 'pe' and not ops[d][4]:
                        continue
                    key, val = tok[d]
                    wait(key, val)
                ins = fn(eobj)
                if dma:
                    ins.then_inc(semof(tok[i][0]), 16)
                elif signal[i]:
                    ins.then_inc(st.sem[me], 1)
            if final and me == 'sp':
                for e in st.engs:
                    wait(('eng', e), end_count[e])
                for ri in range(NRING):
                    wait(('ring', ri), end_ring[ri])

        with nc.Block() as block:
            @block.tensor
            def _(e):
                run_engine('pe', e)

            @block.scalar
            def _(e):
                run_engine('act', e)

            @block.vector
            def _(e):
                run_engine('dve', e)

            @block.gpsimd
            def _(e):
                run_engine('pool', e)

            @block.sync
            def _(e):
                run_engine('sp', e)


def table_layout(cfg):
    H, NT, NPRE, E = cfg.H, cfg.NT, cfg.NPRE, cfg.E
    items = [('ident', 128), ('ropec', NT * 64), ('ropes', NT * 64), ('prec', NPRE * 64), ('pres', NPRE * 64),
             ('dec', 2 * 2 * H), ('kdecpre', NPRE * H), ('gL', 2 * H), ('maskT', 2 * 128), ('convm', 3 * 4 * 128),
             ('seqmask', 16), ('U', 128), ('ONES', 128), ('iotac', cfg.CAP), ('iotaE', E), ('tokid', NT)]
    off, o = {}, 0
    for k, sz in items:
        off[k] = (o, sz)
        o += sz
    return off, o


def make_tables(cfg, half):
    H, NT, NPRE, E, TP = cfg.H, cfg.NT, cfg.NPRE, cfg.E, cfg.TP
    off, tot = table_layout(cfg)
    T = np.zeros((128, tot), np.float32)

    def put(name, arr):
        o, sz = off[name]
        T[:, o:o + sz] = np.asarray(arr, np.float32).reshape(128, sz)
    p = np.arange(128)
    put('ident', np.eye(128))
    inv = (np.float32(ROPE_BASE) ** (-np.arange(64, dtype=np.float32) / np.float32(64))).astype(np.float32)
    pos = np.zeros((128, NT), np.float32)
    for t in range(TP):
        pos[:, t] = half * TP * 128 + t * 128 + p
    pos[:, TP] = PAST_LEN + (p % DEC_SEQ)
    ang = (pos[:, :, None] * inv[None, None, :]).astype(np.float32).astype(np.float64)
    put('ropec', np.cos(ang))
    put('ropes', np.sin(ang))
    posp = (np.arange(NPRE)[None, :] * 128 + p[:, None]).astype(np.float32)
    angp = (posp[:, :, None] * inv[None, None, :]).astype(np.float32).astype(np.float64)
    put('prec', np.cos(angp))
    put('pres', np.sin(angp))
    hh = np.arange(H, dtype=np.float64)
    logg = np.log1p(-np.exp2(-5.0 - hh))
    sc = 128.0 ** -0.5
    dec = np.zeros((128, 2, 2 * H))
    i0 = p.astype(np.float64)
    i1 = (p % DEC_SEQ).astype(np.float64)
    for ty, ii in ((0, i0), (1, i1)):
        dec[:, ty, :H] = np.exp(logg[None, :] * (ii[:, None] + 1.0))
        dec[:, ty, H:] = np.exp(-logg[None, :] * (ii[:, None] + 1.0)) * sc
    put('dec', dec)
    L = NPRE * 128
    j = (np.arange(NPRE)[None, :] * 128 + p[:, None]).astype(np.float64)
    kd = np.exp(logg[None, None, :] * (L - 1.0 - j)[:, :, None]) * sc * (1.0 if half == 1 else 0.0)
    put('kdecpre', kd)
    gL = np.zeros((128, 2, H))
    gL[:, 0, :] = np.exp(logg * 128.0)[None, :]
    gL[:, 1, :] = np.exp(logg * float(DEC_SEQ))[None, :]
    put('gL', gL)
    jj, ii = p[:, None], p[None, :]
    m = np.zeros((128, 2, 128))
    m[:, 0, :] = (jj <= ii)
    m[:, 1, :] = (jj <= ii) & (jj // DEC_SEQ == ii // DEC_SEQ)
    put('maskT', m)
    cm = np.zeros((128, 3, 4, 128))
    flag = 1.0 if half == 1 else 0.0
    for ty in range(2):
        cm[:, ty, 0, :] = (jj == ii - 1)
        cm[:, ty, 1, :] = (jj == ii - 2)
        f = flag if ty == 0 else 1.0
        cm[127, ty, 2, 0] = f
        cm[126, ty, 3, 0] = f
        cm[127, ty, 3, 1] = f
    same = (jj // DEC_SEQ == ii // DEC_SEQ)
    cm[:, 2, 0, :] = (jj == ii - 1) & same
    cm[:, 2, 1, :] = (jj == ii - 2) & same
    for b in range(SEQ_PER_CORE):
        cm[2 * b + 1, 2, 2, 8 * b + 0] = 1.0
        cm[2 * b + 0, 2, 3, 8 * b + 0] = 1.0
        cm[2 * b + 1, 2, 3, 8 * b + 1] = 1.0
    put('convm', cm)
    put('seqmask', (p[:, None] // DEC_SEQ == np.arange(16)[None, :]))
    put('U', (jj < ii))
    put('ONES', np.ones((128, 128)))
    put('iotac', np.tile(np.arange(cfg.CAP)[None, :], (128, 1)))
    put('iotaE', np.tile(np.arange(E)[None, :], (128, 1)))
    put('tokid', np.arange(NT)[None, :] * 128 + p[:, None])
    return T


def build_program(cfg, debug=False):
    D, KD, RW, CW, H, IN, E, F = cfg.D, cfg.KD, cfg.RW, cfg.CW, cfg.H, cfg.IN, cfg.E, cfg.F
    TP, NPRE, NT, CB, CAP, NS, GW, KH, NTOK = cfg.TP, cfg.NPRE, cfg.NT, cfg.CB, cfg.CAP, cfg.NS, cfg.GW, cfg.KH, cfg.NTOK
    toff, ttot = table_layout(cfg)
    nc = bass.Bass("TRN2", target_bir_lowering=False)

    def din(name, shape, dt=F32):
        return nc.dram_tensor(name, list(shape), dt, kind="ExternalInput").ap()

    def dout(name, shape, dt=F32):
        return nc.dram_tensor(name, list(shape), dt, kind="ExternalOutput").ap()

    def dscr(name, shape, dt=F32):
        return nc.dram_tensor(name, list(shape), dt, kind="Internal").ap()

    xm = din("xm", [NTOK, D])
    xp = din("xp", [NPRE * 128, D])
    c32 = din("c32", [32, D])
    sret = din("sret", [SEQ_PER_CORE, H, 128, 128])
    sconv = din("sconv", [32, CW])
    tab = din("tab", [128, ttot])
    w_ada = din("w_ada", [D, 6 * D])
    b_ada = din("b_ada", [1, 6 * D])
    norm1_g = din("norm1_g", [1, D])
    norm2_g = din("norm2_g", [1, D])
    w_in = din("w_in", [D, IN])
    conv_w = din("conv_w", [1, 3 * CW])
    ret_gn = din("ret_gn", [1, RW])
    w_o = din("w_o", [D, D])
    w_router = din("w_router", [D, E])
    b_router = din("b_router", [1, E])
    w_gu = din("w_gu", [E, D, 2 * F])
    b_gu = din("b_gu", [E, 2 * F])
    w_dn = din("w_dn", [E, F, D])
    b_dn = din("b_dn", [E, D])
    final_g = din("final_g", [1, D])

    y = dout("y", [NTOK, D])
    ret_p = dout("ret_p", [H, 128, 128])
    conv_p = dout("conv_p", [2, CW])
    ret_s = dout("ret_s", [SEQ_PER_CORE, H, 128, 128])
    conv_s = dout("conv_s", [128, CW])

    MODD = dscr("MODD", [32, 6 * D])
    PROJ = dscr("PROJ", [NTOK, IN])
    MIX = dscr("MIX", [NTOK, D])
    X1 = dscr("X1", [NTOK, D])
    H2 = dscr("H2", [NTOK, D])
    YS = dscr("YS", [E * CAP, D])
    dbg = {}
    if debug:
        dbg['modd'] = dout("dbg_modd", [32, 6 * D])
        dbg['proj'] = dout("dbg_proj", [NTOK, IN])
        dbg['mix'] = dout("dbg_mix", [NTOK, D])
        dbg['x1'] = dout("dbg_x1", [NTOK, D])
        dbg['h2'] = dout("dbg_h2", [NTOK, D])
        dbg['logits'] = dout("dbg_logits", [128, NT * E])
        dbg['spre'] = dout("dbg_spre", [128, H * 128])
        dbg['gidx'] = dout("dbg_gidx", [128, NT * TOPK], I32)
        dbg['wk'] = dout("dbg_wk", [128, NT * TOPK])
        dbg['idx'] = dout("dbg_idx", [128, E * NS], I32)
        dbg['ys'] = dout("dbg_ys", [E * CAP, D])

    with ExitStack() as top:
        st = SyncState(nc, top)

        def sb(es, name, shape, dt=F32):
            return es.enter_context(nc.sbuf_tensor(name, list(shape), dt))

        def psb(es, name):
            return es.enter_context(nc.psum_tensor(name, [128, 512], F32))

        tabs = sb(top, "tabs", [128, ttot])
        S = sb(top, "S", [128, H, 128])
        zprev = sb(top, "zprev", [128, CW])
        logits = sb(top, "logits", [128, NT, E])
        idx_i = sb(top, "idx_i", [128, E * NS], I32)
        gidx_i = sb(top, "gidx_i", [128, NT * TOPK], I32)
        wk = sb(top, "wk", [128, NT, TOPK])
        ones1 = sb(top, "ones1", [1, 128])
        ones1r = sb(top, "ones1r", [1, 128])

        def tb(name):
            o, sz = toff[name]
            return tabs[:, o:o + sz]
        ident = tb('ident')

        def r32(ap):
            return ap.bitcast(F32R)

        def bc_mid(ap2, n):
            return ap2.unsqueeze(1).to_broadcast([ap2.shape[0], n, ap2.shape[1]])

        def bc_last(ap2, n):
            return ap2.unsqueeze(2).to_broadcast([ap2.shape[0], ap2.shape[1], n])

        with ExitStack() as es:
            P = Prog(nc, st)
            csb = sb(es, "csb", [32, D])
            cT = sb(es, "cT", [128, KD, 32])
            modsb = sb(es, "modsb", [32, 6 * D])
            wab = [sb(es, "wab%d" % i, [128, KD, CB]) for i in range(2)]
            bab = [sb(es, "bab%d" % i, [32, CB]) for i in range(2)]
            ng = sb(es, "ng", [32, 2, D])
            pst = psb(es, "p0t")
            pm = [psb(es, "p0m%d" % i) for i in range(2)]
            P.add('sp', lambda e: e.dma_start(out=tabs[:], in_=tab), w=['tabs'], dma=True)
            P.add('sp', lambda e: e.dma_start(out=csb[:], in_=c32), w=['csb'], dma=True)
            P.add('pool', lambda e: e.memset(ones1[:], 1.0), w=['ones1'])
            P.add('act', lambda e: e.activation(out=r32(ones1r[:]), in_=ones1[:], func=AF.Copy), r=['ones1'], w=['ones1r'])
            P.add('act', lambda e: e.activation(out=csb[:], in_=csb[:], func=AF.Silu), r=['csb'], w=['csb'])
            for k in range(KD):
                P.add('pe', lambda e, k=k: e.transpose(pst[:, k * 32:(k + 1) * 32], csb[:, k * 128:(k + 1) * 128], ident[0:32, 0:32]),
                      r=['csb', 'tabs'], w=['pst'])
            P.add('dve', lambda e: e.tensor_copy(out=r32(cT[:].rearrange("p k m -> p (k m)")), in_=pst[:, 0:KD * 32]), r=['pst'], w=['cT'])
            P.add('sp', lambda e: e.dma_start(out=ng[:, 0, :], in_=norm1_g[0, :].partition_broadcast(32)), w=['ng0'], dma=True)
            P.add('sp', lambda e: e.dma_start(out=ng[:, 1, :], in_=norm2_g[0, :].partition_broadcast(32)), w=['ng1'], dma=True)
            war = w_ada.rearrange("(k p) n -> p k n", p=128)
            NCB = 6 * D // CB
            for cb in range(NCB):
                wb, bb, pp = wab[cb % 2], bab[cb % 2], pm[cb % 2]
                P.add('pool', lambda e, wb=wb, cb=cb: e.dma_start(out=r32(wb[:]), in_=war[:, :, cb * CB:(cb + 1) * CB]), w=[('wab', cb % 2)], dma=True)
                P.add('act', lambda e, bb=bb, cb=cb: e.dma_start(out=bb[:], in_=b_ada[0, cb * CB:(cb + 1) * CB].partition_broadcast(32)),
                      w=[('bab', cb % 2)], dma=True)
                for k in range(KD):
                    P.add('pe', lambda e, k=k, wb=wb, pp=pp: e.matmul(pp[0:32, 0:CB], r32(cT[:, k, :]), r32(wb[:, k, :]), start=(k == 0), stop=(k == KD - 1)),
                          r=['cT', ('wab', cb % 2)], w=[('pm', cb % 2)])
                P.add('dve', lambda e, pp=pp, bb=bb, cb=cb: e.tensor_tensor(out=modsb[:, cb * CB:(cb + 1) * CB], in0=pp[0:32, 0:CB], in1=bb[:], op=ALU.add),
                      r=[('pm', cb % 2), ('bab', cb % 2)], w=[('mod', cb * CB // D)])
            P.add('dve', lambda e: e.scalar_tensor_tensor(out=modsb[:, D:2 * D], in0=modsb[:, D:2 * D], scalar=1.0, in1=ng[:, 0, :], op0=ALU.add, op1=ALU.mult),
                  r=[('mod', 1), 'ng0'], w=[('mod', 1)])
            P.add('dve', lambda e: e.scalar_tensor_tensor(out=modsb[:, 4 * D:5 * D], in0=modsb[:, 4 * D:5 * D], scalar=1.0, in1=ng[:, 1, :], op0=ALU.add, op1=ALU.mult),
                  r=[('mod', 4), 'ng1'], w=[('mod', 4)])
            P.add('sp', lambda e: e.dma_start(out=MODD, in_=modsb[:]), r=[('mod', i) for i in range(6)], w=['MODD'], dma=True)
            if debug:
                P.add('sp', lambda e: e.dma_start(out=dbg['modd'], in_=modsb[:]), r=[('mod', i) for i in range(6)], w=['dbgmodd'], dma=True)
            P.emit()

        def load_mod(P, tile_, key, sec, ty, q='act'):
            if ty == 0:
                P.add(q, lambda e: e.dma_start(out=tile_[:], in_=MODD[0, sec * D:(sec + 1) * D].partition_broadcast(128)), w=[key], dma=True)
            else:
                for b in range(SEQ_PER_CORE):
                    P.add(q, lambda e, b=b: e.dma_start(out=tile_[8 * b:8 * b + 8, :], in_=MODD[1 + b, sec * D:(sec + 1) * D].partition_broadcast(8)),
                          w=[key], dma=True)

        def norm_mod(P, xsrc_ap, xt, hbuf, ss, Gt, SHt, gkeys, tag, ps_banks, dst_fn, h_to=None):
            P.add('sp', lambda e: e.dma_start(out=xt[:], in_=xsrc_ap), w=[tag + 'x'], dma=True)
            P.add('act', lambda e: e.activation(out=hbuf[:], in_=xt[:], func=AF.Square, accum_out=ss[:, 0:1]), r=[tag + 'x'], w=[tag + 'h', tag + 'ss'])
            P.add('dve', lambda e: e.tensor_scalar(out=ss[:, 1:2], in0=ss[:, 0:1], scalar1=1.0 / D, scalar2=NORM_EPS, op0=ALU.mult, op1=ALU.add),
                  r=[tag + 'ss'], w=[tag + 'ss'])
            P.add('act', lambda e: e.sqrt(ss[:, 3:4], ss[:, 1:2]), r=[tag + 'ss'], w=[tag + 'ss'])
            P.add('dve', lambda e: e.reciprocal(out=ss[:, 2:3], in_=ss[:, 3:4]), r=[tag + 'ss'], w=[tag + 'ss'])
            P.add('dve', lambda e: e.scalar_tensor_tensor(out=hbuf[:], in0=xt[:], scalar=ss[:, 2:3], in1=Gt[:], op0=ALU.mult, op1=ALU.mult),
                  r=[tag + 'x', tag + 'ss', gkeys[0]], w=[tag + 'h'])
            P.add('dve', lambda e: e.tensor_tensor(out=hbuf[:], in0=hbuf[:], in1=SHt[:], op=ALU.add), r=[tag + 'h', gkeys[1]], w=[tag + 'h'])
            if h_to is not None:
                P.add('sp', lambda e: e.dma_start(out=h_to, in_=hbuf[:]), r=[tag + 'h'], w=[tag + 'hdram'], dma=True)
            for k4 in range(0, KD, 4):
                bank, bkey = ps_banks[(k4 // 4) % len(ps_banks)]
                nk = min(4, KD - k4)
                for kk in range(nk):
                    k = k4 + kk
                    P.add('pe', lambda e, k=k, kk=kk, bank=bank: e.transpose(bank[:, kk * 128:(kk + 1) * 128], hbuf[:, k * 128:(k + 1) * 128], ident),
                          r=[tag + 'h', 'tabs'], w=[bkey])
                dst, dkey = dst_fn(k4, nk)
                P.add('act', lambda e, bank=bank, dst=dst, nk=nk: e.activation(out=r32(dst), in_=bank[:, 0:nk * 128].rearrange("p (k t) -> p k t", k=nk), func=AF.Copy),
                      r=[bkey], w=[dkey])

        with ExitStack() as es:
            P = Prog(nc, st)
            NG1 = 2 if NPRE >= 2 else 1
            TG = NPRE // NG1
            hTp = sb(es, "hTp", [128, KD, TG * 128])
            kvp = sb(es, "kvp", [128, TG, 2 * RW])
            cup = sb(es, "cup", [128, 2 * CW])
            xts = [sb(es, "p1x%d" % i, [128, D]) for i in range(1)]
            hbs = [sb(es, "p1h%d" % i, [128, D]) for i in range(1)]
            sss = [sb(es, "p1s%d" % i, [128, 4]) for i in range(1)]
            G1 = sb(es, "p1G", [128, D])
            SH1 = sb(es, "p1SH", [128, D])
            wib = [sb(es, "p1w%d" % i, [128, KD, CB]) for i in range(2)]
            tmp = sb(es, "p1tmp", [128, 4, H, 64])
            banks = [(psb(es, "p1b%d" % i), ('p1b', i)) for i in range(8)]
            load_mod(P, G1, 'G1', 1, 0)
            load_mod(P, SH1, 'SH1', 0, 0)
            wir = w_in.rearrange("(k p) n -> p k n", p=128)
            o_c, _ = toff['prec']
            o_s, _ = toff['pres']
            o_kd, _ = toff['kdecpre']
            nj = 0
            for gi in range(NG1):
                tl = list(range(gi * TG, (gi + 1) * TG))
                for li, t in enumerate(tl):
                    norm_mod(P, xp[t * 128:(t + 1) * 128, :], xts[0], hbs[0], sss[0], G1, SH1, ('G1', 'SH1'), 'p1_',
                             banks[0:2], lambda k4, nk, li=li: (hTp[:, k4:k4 + nk, li * 128:(li + 1) * 128], ('hTp', li)))
                nkv = 2 * RW // CB
                jobs = [(RW // CB + j, list(range(TG)), j) for j in range(nkv)]
                cu0 = (4 * RW + CW) // CB
                if gi == NG1 - 1:
                    jobs += [(cu0 + j, [TG - 1], j) for j in range(2 * CW // CB)]
                for (cbi, lis, j) in jobs:
                    wb = wib[nj % 2]
                    wkey = ('p1w', nj % 2)
                    is_cu = cbi >= cu0
                    P.add('pool', lambda e, wb=wb, cbi=cbi: e.dma_start(out=r32(wb[:]), in_=wir[:, :, cbi * CB:(cbi + 1) * CB]), w=[wkey], dma=True)
                    for ti, li in enumerate(lis):
                        bank, bkey = banks[4 + (ti % 4)]
                        for k in range(KD):
                            P.add('pe', lambda e, k=k, li=li, wb=wb, bank=bank: e.matmul(bank[:, 0:CB], r32(hTp[:, k, li * 128:(li + 1) * 128]), r32(wb[:, k, :]),
                                                                                     start=(k == 0), stop=(k == KD - 1)),
                                  r=[('hTp', li), wkey], w=[bkey])
                        if is_cu:
                            dst, dk = cup[:, j * CB:(j + 1) * CB], 'cup'
                        else:
                            dst, dk = kvp[:, li, j * CB:(j + 1) * CB], ('kvp', li)
                        P.add('act' if ti % 2 == 0 else 'dve',
                              (lambda e, dst=dst, bank=bank: e.activation(out=dst, in_=bank[:, 0:CB], func=AF.Copy)) if ti % 2 == 0 else
                              (lambda e, dst=dst, bank=bank: e.tensor_copy(out=dst, in_=bank[:, 0:CB])),
                              r=[bkey], w=[dk])
                    nj += 1
                for li, t in enumerate(tl):
                    kk = kvp[:, li, 0:RW].rearrange("p (h two d) -> p h two d", h=H, two=2)
                    x1, x2 = kk[:, :, 0, :], kk[:, :, 1, :]
                    cs = bc_mid(tabs[:, o_c + t * 64:o_c + (t + 1) * 64], H)
                    sn = bc_mid(tabs[:, o_s + t * 64:o_s + (t + 1) * 64], H)
                    kv = ('kvp', li)
                    P.add('dve', lambda e, x1=x1, cs=cs: e.tensor_tensor(out=tmp[:, 0], in0=x1, in1=cs, op=ALU.mult), r=[kv, 'tabs'], w=['t0'])
                    P.add('pool', lambda e, x2=x2, sn=sn: e.tensor_tensor(out=tmp[:, 1], in0=x2, in1=sn, op=ALU.mult), r=[kv, 'tabs'], w=['t1'])
                    P.add('dve', lambda e, x1=x1, sn=sn: e.tensor_tensor(out=tmp[:, 2], in0=x1, in1=sn, op=ALU.mult), r=[kv, 'tabs'], w=['t2'])
                    P.add('pool', lambda e, x2=x2, cs=cs: e.tensor_tensor(out=tmp[:, 3], in0=x2, in1=cs, op=ALU.mult), r=[kv, 'tabs'], w=['t3'])
                    P.add('dve', lambda e, x1=x1: e.tensor_tensor(out=x1, in0=tmp[:, 0], in1=tmp[:, 1], op=ALU.subtract), r=['t0', 't1'], w=[kv])
                    P.add('dve', lambda e, x2=x2: e.tensor_tensor(out=x2, in0=tmp[:, 2], in1=tmp[:, 3], op=ALU.add), r=['t2', 't3'], w=[kv])
                    k3 = kvp[:, li, 0:RW].rearrange("p (h d) -> p h d", h=H)
                    kd = bc_last(tabs[:, o_kd + t * H:o_kd + (t + 1) * H], 128)
                    P.add('dve', lambda e, k3=k3, kd=kd: e.tensor_tensor(out=k3, in0=k3, in1=kd, op=ALU.mult), r=[kv, 'tabs'], w=[kv])
                sb_banks = banks[2:4]
                for h in range(H):
                    bank, bkey = sb_banks[(h // 4) % 2]
                    for li in range(TG):
                        P.add('pe', lambda e, h=h, li=li, bank=bank: e.matmul(bank[:, (h % 4) * 128:(h % 4 + 1) * 128], kvp[:, li, h * 128:(h + 1) * 128],
                                                                          kvp[:, li, RW + h * 128:RW + (h + 1) * 128], start=(li == 0), stop=(li == TG - 1)),
                              r=[('kvp', li)], w=[bkey])
                    if (h % 4 == 3) or h == H - 1:
                        g = h // 4
                        nh = h - 4 * g + 1
                        sv = S[:, 4 * g:4 * g + nh, :].rearrange("p h e -> p (h e)")
                        if gi == 0:
                            P.add('act', lambda e, bank=bank, sv=sv, nh=nh: e.activation(out=sv, in_=bank[:, 0:nh * 128], func=AF.Copy), r=[bkey], w=['S'])
                        else:
                            P.add('dve', lambda e, bank=bank, sv=sv, nh=nh: e.tensor_tensor(out=sv, in0=bank[:, 0:nh * 128], in1=sv, op=ALU.add), r=[bkey, 'S'], w=['S'])
            P.add('dve', lambda e: e.tensor_tensor(out=zprev[:], in0=cup[:, 0:CW], in1=cup[:, CW:2 * CW], op=ALU.mult), r=['cup'], w=['zprev'])
            if debug:
                P.add('sp', lambda e: e.dma_start(out=dbg['spre'], in_=S[:].rearrange("p h e -> p (h e)")), r=['S'], w=['dbgspre'], dma=True)
            P.emit()

        with ExitStack() as es:
            P = Prog(nc, st)
            hTm = sb(es, "hTm", [128, KD, NTOK])
            xts = [sb(es, "p2x%d" % i, [128, D]) for i in range(1)]
            hbs = [sb(es, "p2h%d" % i, [128, D]) for i in range(1)]
            sss = [sb(es, "p2s%d" % i, [128, 4]) for i in range(1)]
            Gt = [sb(es, "p2G%d" % i, [128, D]) for i in range(2)]
            SHt = [sb(es, "p2SH%d" % i, [128, D]) for i in range(2)]
            wib = [sb(es, "p2w%d" % i, [128, KD, CB]) for i in range(2)]
            stg = [sb(es, "p2st%d" % i, [128, CB]) for i in range(4)]
            banks = [(psb(es, "p2b%d" % i), ('p2b', i)) for i in range(8)]
            for ty in range(2):
                load_mod(P, Gt[ty], ('G', ty), 1, ty)
                load_mod(P, SHt[ty], ('SH', ty), 0, ty)
            for t in range(NT):
                i2 = 0
                ty = 0 if t < TP else 1
                norm_mod(P, xm[t * 128:(t + 1) * 128, :], xts[i2], hbs[i2], sss[i2], Gt[ty], SHt[ty], (('G', ty), ('SH', ty)), 'p2%d' % i2,
                         banks[0:4], lambda k4, nk, t=t: (hTm[:, k4:k4 + nk, t * 128:(t + 1) * 128], ('hTm', t)))
            wir = w_in.rearrange("(k p) n -> p k n", p=128)
            cnt = 0
            for cbi in range(IN // CB):
                wb = wib[cbi % 2]
                wkey = ('p2w', cbi % 2)
                P.add('pool', lambda e, wb=wb, cbi=cbi: e.dma_start(out=r32(wb[:]), in_=wir[:, :, cbi * CB:(cbi + 1) * CB]), w=[wkey], dma=True)
                for t in range(NT):
                    bank, bkey = banks[4 + (cnt % 4)]
                    sg_, skey = stg[cnt % 4], ('stg', cnt % 4)
                    for k in range(KD):
                        P.add('pe', lambda e, k=k, t=t, wb=wb, bank=bank: e.matmul(bank[:, 0:CB], r32(hTm[:, k, t * 128:(t + 1) * 128]), r32(wb[:, k, :]),
                                                                                 start=(k == 0), stop=(k == KD - 1)),
                              r=[('hTm', t), wkey], w=[bkey])
                    if cnt % 2 == 0:
                        P.add('act', lambda e, sg_=sg_, bank=bank: e.activation(out=sg_[:], in_=bank[:, 0:CB], func=AF.Copy), r=[bkey], w=[skey])
                    else:
                        P.add('dve', lambda e, sg_=sg_, bank=bank: e.tensor_copy(out=sg_[:], in_=bank[:, 0:CB]), r=[bkey], w=[skey])
                    P.add('pool', lambda e, sg_=sg_, t=t, cbi=cbi: e.dma_start(out=PROJ[t * 128:(t + 1) * 128, cbi * CB:(cbi + 1) * CB], in_=sg_[:]),
                          r=[skey], w=[('PROJ', t)], dma=True)
                    cnt += 1
            if debug:
                P.add('sp', lambda e: e.dma_start(out=dbg['proj'], in_=PROJ), r=[('PROJ', t) for t in range(NT)], w=['dbgproj'], dma=True)
            P.emit()

        with ExitStack() as es:
            P = Prog(nc, st)
            pj = sb(es, "pj", [128, IN])
            qkr = sb(es, "qkr", [128, 2 * H, 128])
            tmp = sb(es, "p3tmp", [128, 4, 2 * H, 64])
            qkT = sb(es, "qkT", [128, 2 * H, 128])
            smT = sb(es, "smT", [128, H, 128])
            osb = sb(es, "osb", [128, H, 128])
            osq = sb(es, "osq", [128, H, 128])
            stat = sb(es, "stat", [128, 6, H])
            gnb = sb(es, "gnb", [128, RW])
            cwb = sb(es, "cwb", [128, 3, CW])
            zp_s = sb(es, "zp_s", [128, CW])
            ctmp = sb(es, "ctmp", [128, CW])
            Zq = [sb(es, "Zq%d" % i, [128, SEQ_PER_CORE, 128]) for i in range(2)]
            kbZ = [sb(es, "kbZ%d" % i, [128, RW]) for i in range(2)]
            Sb = [sb(es, "Sb%d" % i, [128, H, 128]) for i in range(2)]
            Sold = [sb(es, "Sold%d" % i, [128, H, 128]) for i in range(2)]
            Ssh = [sb(es, "Ssh%d" % i, [128, SEQ_PER_CORE, 128]) for i in range(2)]
            banks = [(psb(es, "p3b%d" % i), ('p3b', i)) for i in range(8)]
            P.add('act', lambda e: e.dma_start(out=gnb[:], in_=ret_gn[0, :].partition_broadcast(128)), w=['gnb'], dma=True)
            P.add('act', lambda e: e.dma_start(out=cwb[:].rearrange("p a c -> p (a c)"), in_=conv_w[0, :].partition_broadcast(128)), w=['cwb'], dma=True)
            P.add('pool', lambda e: e.memset(zp_s[:], 0.0), w=['zp_s'])
            P.add('act', lambda e: e.dma_start(out=zp_s[0:32, :], in_=sconv), r=['zp_s'], w=['zp_s'], dma=True)
            for i in range(2):
                P.add('pool', lambda e, i=i: e.memset(Zq[i][:], 0.0), w=[('Zq', i)])
            o_c, _ = toff['ropec']
            o_s, _ = toff['ropes']
            o_dec, _ = toff['dec']
            o_gL, _ = toff['gL']
            o_m, _ = toff['maskT']
            o_cm, _ = toff['convm']
            o_sm, _ = toff['seqmask']
            QO, KO, VO, GO, BO, CO, UO = 0, RW, 2 * RW, 3 * RW, 4 * RW, 4 * RW + CW, 4 * RW + 2 * CW
            nHG = (H + 3) // 4
            for t in range(NT):
                ty = 0 if t < TP else 1
                cty = (0 if t == 0 else 1) if ty == 0 else 2
                P.add('sp', lambda e, t=t: e.dma_start(out=pj[:], in_=PROJ[t * 128:(t + 1) * 128, :]), w=['pj_q', 'pj_v', 'pj_g', 'pj_B', 'pj_C', 'pj_u'], dma=True)
                qk = pj[:, 0:2 * RW].rearrange("p (h two d) -> p h two d", h=2 * H, two=2)
                x1, x2 = qk[:, :, 0, :], qk[:, :, 1, :]
                ov = qkr[:].rearrange("p h (two d) -> p h two d", two=2)
                cs = bc_mid(tabs[:, o_c + t * 64:o_c + (t + 1) * 64], 2 * H)
                sn = bc_mid(tabs[:, o_s + t * 64:o_s + (t + 1) * 64], 2 * H)
                P.add('dve', lambda e, x1=x1, cs=cs: e.tensor_tensor(out=tmp[:, 0], in0=x1, in1=cs, op=ALU.mult), r=['pj_q', 'tabs'], w=['t0'])
                P.add('pool', lambda e, x2=x2, sn=sn: e.tensor_tensor(out=tmp[:, 1], in0=x2, in1=sn, op=ALU.mult), r=['pj_q', 'tabs'], w=['t1'])
                P.add('dve', lambda e, x1=x1, sn=sn: e.tensor_tensor(out=tmp[:, 2], in0=x1, in1=sn, op=ALU.mult), r=['pj_q', 'tabs'], w=['t2'])
                P.add('pool', lambda e, x2=x2, cs=cs: e.tensor_tensor(out=tmp[:, 3], in0=x2, in1=cs, op=ALU.mult), r=['pj_q', 'tabs'], w=['t3'])
                P.add('dve', lambda e, ov=ov: e.tensor_tensor(out=ov[:, :, 0, :], in0=tmp[:, 0], in1=tmp[:, 1], op=ALU.subtract), r=['t0', 't1'], w=['qkr'])
                P.add('dve', lambda e, ov=ov: e.tensor_tensor(out=ov[:, :, 1, :], in0=tmp[:, 2], in1=tmp[:, 3], op=ALU.add), r=['t2', 't3', 'qkr'], w=['qkr'])
                dc = bc_last(tabs[:, o_dec + ty * 2 * H:o_dec + (ty + 1) * 2 * H], 128)
                P.add('dve', lambda e, dc=dc: e.tensor_tensor(out=qkr[:], in0=qkr[:], in1=dc, op=ALU.mult), r=['qkr', 'tabs'], w=['qkr'])
                for g in range((2 * H + 3) // 4):
                    bank, bkey = banks[g % 4]
                    nh = min(4, 2 * H - 4 * g)
                    for hh in range(nh):
                        P.add('pe', lambda e, g=g, hh=hh, bank=bank: e.transpose(bank[:, hh * 128:(hh + 1) * 128], qkr[:, 4 * g + hh, :], ident),
                              r=['qkr', 'tabs'], w=[bkey])
                    P.add('act', lambda e, g=g, nh=nh, bank=bank: e.activation(out=qkT[:, 4 * g:4 * g + nh, :].rearrange("p h t -> p (h t)"), in_=bank[:, 0:nh * 128], func=AF.Copy),
                          r=[bkey], w=['qkT'])
                mk = bc_mid(tabs[:, o_m + ty * 128:o_m + (ty + 1) * 128], 4)
                for g in range(nHG):
                    bank, bkey = banks[4 + g % 2]
                    nh = min(4, H - 4 * g)
                    for hh in range(nh):
                        h = 4 * g + hh
                        P.add('pe', lambda e, h=h, hh=hh, bank=bank: e.matmul(bank[:, hh * 128:(hh + 1) * 128], qkT[:, H + h, :], qkT[:, h, :], start=True, stop=True),
                              r=['qkT'], w=[bkey])
                    P.add('dve', lambda e, g=g, nh=nh, bank=bank, mk=mk: e.tensor_tensor(out=smT[:, 4 * g:4 * g + nh, :], in0=bank[:, 0:nh * 128].rearrange("p (h t) -> p h t", h=nh),
                                                                                    in1=mk[:, 0:nh, :], op=ALU.mult),
                          r=[bkey, 'tabs'], w=['smT'])
                obanks = [banks[6], banks[7]]
                for h in range(H):
                    bank, bkey = obanks[(h // 4) % 2]
                    oreg = bank[:, (h % 4) * 128:(h % 4 + 1) * 128]
                    vh = pj[:, VO + h * 128:VO + (h + 1) * 128]
                    if ty == 0:
                        P.add('pe', lambda e, h=h, oreg=oreg, vh=vh: e.matmul(oreg, smT[:, h, :], vh, start=True, stop=False), r=['smT', 'pj_v'], w=[bkey])
                        P.add('pe', lambda e, h=h, oreg=oreg: e.matmul(oreg, qkT[:, h, :], S[:, h, :], start=False, stop=True), r=['qkT', 'S'], w=[bkey])
                    else:
                        zq, zk = Zq[h % 2], ('Zq', h % 2)
                        ssh, sshk = Ssh[h % 2], ('Ssh', h % 2)
                        P.add('act', lambda e, h=h, ssh=ssh: e.dma_start(out=ssh[:], in_=sret[:, h, :, :].rearrange("b d e -> d b e")), w=[sshk], dma=True)
                        P.add('dve', lambda e, h=h, zq=zq: e.tensor_copy(
                            out=bass.AP(zq[:].tensor, zq[:].offset, [list(zq[:].ap[0]), [136, SEQ_PER_CORE], [1, 8]]),
                            in_=qkT[:, h, :].rearrange("p (b i) -> p b i", i=8)), r=['qkT', zk], w=[zk])
                        P.add('pe', lambda e, h=h, oreg=oreg, vh=vh: e.matmul(oreg, smT[:, h, :], vh, start=True, stop=False), r=['smT', 'pj_v'], w=[bkey])
                        for b in range(SEQ_PER_CORE):
                            P.add('pe', lambda e, h=h, b=b, oreg=oreg, zq=zq, ssh=ssh: e.matmul(oreg, zq[:, b, :], ssh[:, b, :], start=False, stop=(b == SEQ_PER_CORE - 1)),
                                  r=[zk, sshk], w=[bkey])
                    if (h % 4 == 3) or h == H - 1:
                        g = h // 4
                        nh = h - 4 * g + 1
                        P.add('act', lambda e, g=g, nh=nh, bank=bank: e.activation(out=osb[:, 4 * g:4 * g + nh, :].rearrange("p h e -> p (h e)"), in_=bank[:, 0:nh * 128], func=AF.Copy),
                              r=[bkey], w=['osb'])
                if ty == 0:
                    for g in range(nHG):
                        bank, bkey = banks[4 + g % 2]
                        nh = min(4, H - 4 * g)
                        for hh in range(nh):
                            h = 4 * g + hh
                            P.add('pe', lambda e, h=h, hh=hh, bank=bank: e.matmul(bank[:, hh * 128:(hh + 1) * 128], qkr[:, H + h, :], pj[:, VO + h * 128:VO + (h + 1) * 128], start=True, stop=True),
                                  r=['qkr', 'pj_v'], w=[bkey])
                        P.add('dve', lambda e, g=g, nh=nh, bank=bank: e.tensor_tensor(out=S[:, 4 * g:4 * g + nh, :], in0=bank[:, 0:nh * 128].rearrange("p (h t) -> p h t", h=nh),
                                                                                in1=S[:, 4 * g:4 * g + nh, :], op=ALU.add), r=[bkey, 'S'], w=['S'])
                    gl = bc_last(tabs[:, o_gL:o_gL + H], 128)
                    P.add('dve', lambda e, gl=gl: e.tensor_tensor(out=S[:], in0=S[:], in1=gl, op=ALU.mult), r=['S', 'tabs'], w=['S'])
                    if t == TP - 1:
                        P.add('sp', lambda e: e.dma_start(out=ret_p.rearrange("h d e -> d h e"), in_=S[:]), r=['S'], w=['ret_p'], dma=True)
                else:
                    gl = bc_last(tabs[:, o_gL + H:o_gL + 2 * H], 128)
                    for b in range(SEQ_PER_CORE):
                        kz, kzk = kbZ[b % 2], ('kbZ', b % 2)
                        sbb, sbk = Sb[b % 2], ('Sb', b % 2)
                        so, sok = Sold[b % 2], ('Sold', b % 2)
                        P.add('act', lambda e, b=b, so=so: e.dma_start(out=so[:], in_=sret[b].rearrange("h d e -> d h e")), w=[sok], dma=True)
                        P.add('pool', lambda e, b=b, kz=kz: e.tensor_scalar(out=kz[:], in0=qkr[:, H:2 * H, :].rearrange("p h d -> p (h d)"),
                                                                            scalar1=tabs[:, o_sm + b:o_sm + b + 1], scalar2=None, op0=ALU.mult),
                              r=['qkr', 'tabs'], w=[kzk])
                        for g in range(nHG):
                            bank, bkey = banks[4 + g % 2]
                            nh = min(4, H - 4 * g)
                            for hh in range(nh):
                                h = 4 * g + hh
                                P.add('pe', lambda e, h=h, hh=hh, bank=bank, kz=kz: e.matmul(bank[:, hh * 128:(hh + 1) * 128], kz[:, h * 128:(h + 1) * 128],
                                                                                     pj[:, VO + h * 128:VO + (h + 1) * 128], start=True, stop=True),
                                      r=[kzk, 'pj_v'], w=[bkey])
                            P.add('dve', lambda e, g=g, nh=nh, bank=bank, b=b, sbb=sbb, so=so: e.tensor_tensor(out=sbb[:, 4 * g:4 * g + nh, :], in0=bank[:, 0:nh * 128].rearrange("p (h t) -> p h t", h=nh),
                                                                                              in1=so[:, 4 * g:4 * g + nh, :], op=ALU.add),
                                  r=[bkey, sok], w=[sbk])
                        P.add('dve', lambda e, sbb=sbb, gl=gl: e.tensor_tensor(out=sbb[:], in0=sbb[:], in1=gl, op=ALU.mult), r=[sbk, 'tabs'], w=[sbk])
                        P.add('sp', lambda e, b=b, sbb=sbb: e.dma_start(out=ret_s[b].rearrange("h d e -> d h e"), in_=sbb[:]), r=[sbk], w=[('ret_s', b)], dma=True)
                P.add('dve', lambda e: e.tensor_reduce(out=stat[:, 0, :], in_=osb[:], axis=AX.X, op=ALU.add), r=['osb'], w=['st0'])
                P.add('pool', lambda e: e.tensor_tensor(out=osq[:], in0=osb[:], in1=osb[:], op=ALU.mult), r=['osb'], w=['osq'])
                P.add('dve', lambda e: e.tensor_reduce(out=stat[:, 1, :], in_=osq[:], axis=AX.X, op=ALU.add), r=['osq'], w=['st1'])
                P.add('dve', lambda e: e.tensor_scalar(out=stat[:, 2, :], in0=stat[:, 0, :], scalar1=1.0 / 128, scalar2=None, op0=ALU.mult), r=['st0'], w=['st2'])
                P.add('dve', lambda e: e.tensor_tensor(out=stat[:, 3, :], in0=stat[:, 2, :], in1=stat[:, 2, :], op=ALU.mult), r=['st2'], w=['st3'])
                P.add('dve', lambda e: e.scalar_tensor_tensor(out=stat[:, 4, :], in0=stat[:, 1, :], scalar=1.0 / 128, in1=stat[:, 3, :], op0=ALU.mult, op1=ALU.subtract),
                      r=['st1', 'st3'], w=['st4'])
                P.add('dve', lambda e: e.tensor_scalar(out=stat[:, 5, :], in0=stat[:, 4, :], scalar1=NORM_EPS, scalar2=None, op0=ALU.add), r=['st4'], w=['st5'])
                P.add('act', lambda e: e.sqrt(stat[:, 5, :], stat[:, 5, :]), r=['st5'], w=['st5'])
                P.add('dve', lambda e: e.reciprocal(out=stat[:, 5, :], in_=stat[:, 5, :]), r=['st5'], w=['st5'])
                P.add('dve', lambda e: e.tensor_tensor(out=osb[:], in0=osb[:], in1=bc_last(stat[:, 2, :], 128), op=ALU.subtract), r=['osb', 'st2', 'osq'], w=['osb'])
                P.add('dve', lambda e: e.tensor_tensor(out=osb[:], in0=osb[:], in1=bc_last(stat[:, 5, :], 128), op=ALU.mult), r=['osb', 'st5'], w=['osb'])
                P.add('pool', lambda e: e.tensor_tensor(out=osb[:].rearrange("p h e -> p (h e)"), in0=osb[:].rearrange("p h e -> p (h e)"), in1=gnb[:], op=ALU.mult), r=['osb', 'gnb'], w=['osb'])
                P.add('act', lambda e: e.activation(out=pj[:, GO:GO + RW], in_=pj[:, GO:GO + RW], func=AF.Silu), r=['pj_g'], w=['pj_g'])
                P.add('dve', lambda e: e.tensor_tensor(out=pj[:, GO:GO + RW], in0=pj[:, GO:GO + RW], in1=osb[:].rearrange("p h e -> p (h e)"), op=ALU.mult), r=['pj_g', 'osb'], w=['pj_g'])
                zz = pj[:, CO:CO + CW]
                P.add('pool', lambda e, zz=zz: e.tensor_tensor(out=zz, in0=zz, in1=pj[:, UO:UO + CW], op=ALU.mult), r=['pj_C', 'pj_u'], w=['pj_C'])
                zpv = zprev if ty == 0 else zp_s
                zpk = 'zprev' if ty == 0 else 'zp_s'
                cmv = tabs[:, o_cm + cty * 512:o_cm + (cty + 1) * 512].rearrange("p (a t) -> p a t", a=4)
                shb = [banks[0], banks[1], banks[2], banks[3]]
                nq = (CW + 511) // 512
                for sh in range(2):
                    for q in range(nq):
                        bank, bkey = shb[sh * 2 + q % 2]
                        wq = min(512, CW - q * 512)
                        P.add('pe', lambda e, sh=sh, q=q, wq=wq, bank=bank, cmv=cmv, zz=zz: e.matmul(bank[:, 0:wq], cmv[:, sh, :], zz[:, q * 512:q * 512 + wq], start=True, stop=False),
                              r=['tabs', 'pj_C'], w=[bkey])
                        P.add('pe', lambda e, sh=sh, q=q, wq=wq, bank=bank, cmv=cmv, zpv=zpv: e.matmul(bank[:, 0:wq], cmv[:, 2 + sh, :], zpv[:, q * 512:q * 512 + wq], start=False, stop=True),
                              r=['tabs', zpk], w=[bkey])
                        cs_ = slice(q * 512, q * 512 + wq)
                        if sh == 0:
                            P.add('dve', lambda e, bank=bank, wq=wq, cs_=cs_: e.tensor_tensor(out=ctmp[:, cs_], in0=bank[:, 0:wq], in1=cwb[:, 1, cs_], op=ALU.mult),
                                  r=[bkey, 'cwb'], w=[('ctmp', q)])
                        else:
                            P.add('dve', lambda e, bank=bank, wq=wq, cs_=cs_: e.tensor_tensor(out=pj[:, UO + cs_.start:UO + cs_.stop], in0=bank[:, 0:wq], in1=cwb[:, 0, cs_], op=ALU.mult),
                                  r=[bkey, 'cwb', 'pj_C'], w=[('pj_u2', q)])
                P.add('pool', lambda e: e.tensor_tensor(out=ctmp[:], in0=ctmp[:], in1=pj[:, UO:UO + CW], op=ALU.add), r=[('ctmp', q) for q in range(nq)] + [('pj_u2', q) for q in range(nq)], w=['ctmpf'])
                P.add('dve', lambda e, zz=zz: e.tensor_tensor(out=pj[:, UO:UO + CW], in0=zz, in1=cwb[:, 2, :], op=ALU.mult), r=['pj_C', 'cwb', 'ctmpf'], w=['pj_u'])
                P.add('dve', lambda e: e.tensor_tensor(out=ctmp[:], in0=ctmp[:], in1=pj[:, UO:UO + CW], op=ALU.add), r=['ctmpf', 'pj_u'], w=['ctmpf'])
                P.add('dve', lambda e: e.tensor_tensor(out=pj[:, BO:BO + CW], in0=pj[:, BO:BO + CW], in1=ctmp[:], op=ALU.mult), r=['pj_B', 'ctmpf'], w=['pj_B'])
                if ty == 0:
                    P.add('act', lambda e, zz=zz: e.activation(out=zprev[:], in_=zz, func=AF.Copy), r=['pj_C', 'zprev'], w=['zprev'])
                    if t == TP - 1:
                        P.add('sp', lambda e: e.dma_start(out=conv_p, in_=zprev[126:128, :]), r=['zprev'], w=['conv_p'], dma=True)
                else:
                    P.add('sp', lambda e, zz=zz: e.dma_start(out=conv_s, in_=zz), r=['pj_C'], w=['conv_s'], dma=True)
                P.add('sp', lambda e, t=t: e.dma_start(out=MIX[t * 128:(t + 1) * 128, :], in_=pj[:, GO:GO + RW + CW]), r=['pj_g', 'pj_B'], w=[('MIX', t)], dma=True)
            P.emit()


        with ExitStack() as es:
            P = Prog(nc, st)
            GS = 3
            mixT = sb(es, "mixT", [128, KD, GS * 128])
            mxs = [sb(es, "mxs%d" % i, [128, D]) for i in range(2)]
            x1g = [sb(es, "x1g%d" % i, [128, D]) for i in range(GS)]
            wob = [sb(es, "wob%d" % i, [128, KD, CB]) for i in range(2)]
            g1t = sb(es, "g1t", [128, D])
            G2t = sb(es, "G2t", [128, D])
            SH2t = sb(es, "SH2t", [128, D])
            h2b = [sb(es, "h2b%d" % i, [128, D]) for i in range(2)]
            h2T = [sb(es, "h2T%d" % i, [128, KD, 128]) for i in range(2)]
            ss4 = [sb(es, "ss4%d" % i, [128, 4]) for i in range(2)]
            tmp4 = [sb(es, "tmp4%d" % i, [128, CB]) for i in range(2)]
            wr = sb(es, "wr", [128, KD, E])
            brb = sb(es, "brb", [128, E])
            banks = [(psb(es, "p4b%d" % i), ('p4b', i)) for i in range(8)]
            P.add('act', lambda e: e.dma_start(out=wr[:], in_=w_router.rearrange("(k p) n -> p k n", p=128)), w=['wr'], dma=True)
            P.add('act', lambda e: e.dma_start(out=brb[:], in_=b_router[0, :].partition_broadcast(128)), w=['brb'], dma=True)
            wor = w_o.rearrange("(k p) n -> p k n", p=128)
            groups = [list(range(g0, min(g0 + GS, NT))) for g0 in range(0, NT, GS)]
            cur_ty = -1
            wcnt = 0
            ecnt = 0
            for grp in groups:
                sub = [[t for t in grp if t < TP], [t for t in grp if t >= TP]]
                for ty, tl in enumerate(sub):
                    if not tl:
                        continue
                    if ty != cur_ty:
                        load_mod(P, g1t, 'g1t', 2, ty)
                        load_mod(P, G2t, 'G2t', 4, ty)
                        load_mod(P, SH2t, 'SH2t', 3, ty)
                        cur_ty = ty
                    for li, t in enumerate(tl):
                        ms, mk_ = mxs[li % 2], ('mxs', li % 2)
                        P.add('sp', lambda e, t=t, ms=ms: e.dma_start(out=ms[:], in_=MIX[t * 128:(t + 1) * 128, :]), w=[mk_], dma=True)
                        P.add('sp', lambda e, t=t, li=li: e.dma_start(out=x1g[li][:], in_=xm[t * 128:(t + 1) * 128, :]), w=[('x1g', li)], dma=True)
                        for k4 in range(0, KD, 4):
                            bank, bkey = banks[(k4 // 4) % 4]
                            nk = min(4, KD - k4)
                            for kk in range(nk):
                                P.add('pe', lambda e, k=k4 + kk, kk=kk, bank=bank, ms=ms: e.transpose(bank[:, kk * 128:(kk + 1) * 128], ms[:, k * 128:(k + 1) * 128], ident),
                                      r=[mk_, 'tabs'], w=[bkey])
                            P.add('act', lambda e, bank=bank, k4=k4, nk=nk, li=li: e.activation(out=r32(mixT[:, k4:k4 + nk, li * 128:(li + 1) * 128]),
                                                                                        in_=bank[:, 0:nk * 128].rearrange("p (k t) -> p k t", k=nk), func=AF.Copy),
                                  r=[bkey], w=[('mixT', li)])
                    for cbi in range(D // CB):
                        wb, wkey = wob[wcnt % 2], ('wob', wcnt % 2)
                        wcnt += 1
                        P.add('pool', lambda e, wb=wb, cbi=cbi: e.dma_start(out=r32(wb[:]), in_=wor[:, :, cbi * CB:(cbi + 1) * CB]), w=[wkey], dma=True)
                        for li, t in enumerate(tl):
                            bank, bkey = banks[4 + ecnt % 2]
                            tm, tmk = tmp4[ecnt % 2], ('tmp4', ecnt % 2)
                            ecnt += 1
                            for k in range(KD):
                                P.add('pe', lambda e, k=k, li=li, wb=wb, bank=bank: e.matmul(bank[:, 0:CB], r32(mixT[:, k, li * 128:(li + 1) * 128]), r32(wb[:, k, :]),
                                                                                          start=(k == 0), stop=(k == KD - 1)),
                                      r=[('mixT', li), wkey], w=[bkey])
                            cs_ = slice(cbi * CB, (cbi + 1) * CB)
                            P.add('dve', lambda e, bank=bank, tm=tm, cs_=cs_: e.tensor_tensor(out=tm[:], in0=bank[:, 0:CB], in1=g1t[:, cs_], op=ALU.mult), r=[bkey, 'g1t'], w=[tmk])
                            P.add('pool', lambda e, tm=tm, li=li, cs_=cs_: e.tensor_tensor(out=x1g[li][:, cs_], in0=x1g[li][:, cs_], in1=tm[:], op=ALU.add), r=[tmk, ('x1g', li)], w=[('x1g', li)])
                    for li, t in enumerate(tl):
                        i2 = t % 2
                        xk = ('x1g', li)
                        P.add('sp', lambda e, t=t, li=li: e.dma_start(out=X1[t * 128:(t + 1) * 128, :], in_=x1g[li][:]), r=[xk], w=[('X1', t)], dma=True)
                        hb, hk = h2b[i2], ('h2b', i2)
                        ss, sk = ss4[i2], ('ss4', i2)
                        P.add('act', lambda e, li=li, hb=hb, ss=ss: e.activation(out=hb[:], in_=x1g[li][:], func=AF.Square, accum_out=ss[:, 0:1]), r=[xk], w=[hk, sk])
                        P.add('dve', lambda e, ss=ss: e.tensor_scalar(out=ss[:, 1:2], in0=ss[:, 0:1], scalar1=1.0 / D, scalar2=NORM_EPS, op0=ALU.mult, op1=ALU.add), r=[sk], w=[sk])
                        P.add('act', lambda e, ss=ss: e.sqrt(ss[:, 3:4], ss[:, 1:2]), r=[sk], w=[sk])
                        P.add('dve', lambda e, ss=ss: e.reciprocal(out=ss[:, 2:3], in_=ss[:, 3:4]), r=[sk], w=[sk])
                        P.add('dve', lambda e, li=li, hb=hb, ss=ss: e.scalar_tensor_tensor(out=hb[:], in0=x1g[li][:], scalar=ss[:, 2:3], in1=G2t[:], op0=ALU.mult, op1=ALU.mult),
                              r=[xk, sk, 'G2t'], w=[hk])
                        P.add('dve', lambda e, hb=hb: e.tensor_tensor(out=hb[:], in0=hb[:], in1=SH2t[:], op=ALU.add), r=[hk, 'SH2t'], w=[hk])
                        P.add('sp', lambda e, t=t, hb=hb: e.dma_start(out=H2[t * 128:(t + 1) * 128, :], in_=hb[:]), r=[hk], w=[('H2', t)], dma=True)
                        hT, hTk = h2T[i2], ('h2T', i2)
                        for k4 in range(0, KD, 4):
                            bank, bkey = banks[(k4 // 4) % 4]
                            nk = min(4, KD - k4)
                            for kk in range(nk):
                                P.add('pe', lambda e, k=k4 + kk, kk=kk, bank=bank, hb=hb: e.transpose(bank[:, kk * 128:(kk + 1) * 128], hb[:, k * 128:(k + 1) * 128], ident),
                                      r=[hk, 'tabs'], w=[bkey])
                            P.add('act', lambda e, bank=bank, k4=k4, nk=nk, hT=hT: e.activation(out=hT[:, k4:k4 + nk, :], in_=bank[:, 0:nk * 128].rearrange("p (k t) -> p k t", k=nk), func=AF.Copy),
                                  r=[bkey], w=[hTk])
                        bank, bkey = banks[6 + i2]
                        for k in range(KD):
                            P.add('pe', lambda e, k=k, hT=hT, bank=bank: e.matmul(bank[:, 0:E], hT[:, k, :], wr[:, k, :], start=(k == 0), stop=(k == KD - 1)), r=[hTk, 'wr'], w=[bkey])
                        P.add('dve', lambda e, t=t, bank=bank: e.tensor_tensor(out=logits[:, t, :], in0=bank[:, 0:E], in1=brb[:], op=ALU.add), r=[bkey, 'brb'], w=['logits'])
            if debug:
                P.add('sp', lambda e: e.dma_start(out=dbg['mix'], in_=MIX), w=['dbgmix'], dma=True)
                P.add('sp', lambda e: e.dma_start(out=dbg['x1'], in_=X1), r=[('X1', t) for t in range(NT)], w=['dbgx1'], dma=True)
                P.add('sp', lambda e: e.dma_start(out=dbg['h2'], in_=H2), r=[('H2', t) for t in range(NT)], w=['dbgh2'], dma=True)
                P.add('sp', lambda e: e.dma_start(out=dbg['logits'], in_=logits[:].rearrange("p t e -> p (t e)")), r=['logits'], w=['dbglog'], dma=True)
            P.emit()

        with ExitStack() as es:
            P = Prog(nc, st)
            NE = NT * E
            rem = sb(es, "rem", [128, NT, E])
            oh = [sb(es, "oh%d" % k, [128, NT, E]) for k in range(TOPK)]
            mask = sb(es, "mask", [128, NT, E])
            ex = sb(es, "ex", [128, NT, E])
            gates = sb(es, "gates", [128, NT, E])
            rank = sb(es, "rank", [128, NT, E])
            t3 = sb(es, "t3", [128, NT, E])
            vk = sb(es, "vk", [128, 8, NT])
            t4 = sb(es, "t4", [128, NT])
            gf = sb(es, "gf", [128, NT, TOPK])
            Pm = [sb(es, "Pm%d" % i, [128, NT, CAP]) for i in range(2)]
            idxf = sb(es, "idxf", [128, E * NS])
            banks = [(psb(es, "p5b%d" % i), ('p5b', i)) for i in range(8)]
            o_U, _ = toff['U']
            o_1, _ = toff['ONES']
            o_ic, _ = toff['iotac']
            o_ie, _ = toff['iotaE']
            o_tk, _ = toff['tokid']
            Ut, ONt = tabs[:, o_U:o_U + 128], tabs[:, o_1:o_1 + 128]
            P.add('dve', lambda e: e.tensor_copy(out=rem[:], in_=logits[:]), r=['logits'], w=['rem'])
            P.add('dve', lambda e: e.tensor_reduce(out=vk[:, 4, :], in_=logits[:], axis=AX.X, op=ALU.max), r=['logits'], w=['m1'])
            for k in range(TOPK):
                P.add('dve', lambda e, k=k: e.tensor_reduce(out=vk[:, k, :], in_=rem[:], axis=AX.X, op=ALU.max), r=['rem'], w=[('vk', k)])
                P.add('dve', lambda e, k=k: e.tensor_tensor(out=oh[k][:], in0=rem[:], in1=bc_last(vk[:, k, :], E), op=ALU.is_equal), r=['rem', ('vk', k)], w=[('oh', k)])
                P.add('dve', lambda e, k=k: e.scalar_tensor_tensor(out=rem[:], in0=oh[k][:], scalar=-1e30, in1=rem[:], op0=ALU.mult, op1=ALU.add), r=[('oh', k), 'rem'], w=['rem'])
            P.add('dve', lambda e: e.tensor_tensor(out=mask[:], in0=oh[0][:], in1=oh[1][:], op=ALU.add), r=[('oh', 0), ('oh', 1)], w=['mask'])
            P.add('dve', lambda e: e.tensor_tensor(out=mask[:], in0=mask[:], in1=oh[2][:], op=ALU.add), r=[('oh', 2), 'mask'], w=['mask'])
            P.add('dve', lambda e: e.tensor_tensor(out=mask[:], in0=mask[:], in1=oh[3][:], op=ALU.add), r=[('oh', 3), 'mask'], w=['mask'])
            P.add('dve', lambda e: e.tensor_tensor(out=ex[:], in0=logits[:], in1=bc_last(vk[:, 4, :], E), op=ALU.subtract), r=['logits', 'm1'], w=['ex'])
            P.add('act', lambda e: e.activation(out=ex[:], in_=ex[:], func=AF.Exp), r=['ex'], w=['ex'])
            P.add('dve', lambda e: e.tensor_tensor(out=ex[:], in0=ex[:], in1=mask[:], op=ALU.mult), r=['ex', 'mask'], w=['ex'])
            P.add('dve', lambda e: e.tensor_reduce(out=vk[:, 5, :], in_=ex[:], axis=AX.X, op=ALU.add), r=['ex'], w=['den'])
            P.add('dve', lambda e: e.reciprocal(out=vk[:, 6, :], in_=vk[:, 5, :]), r=['den'], w=['rden'])
            P.add('dve', lambda e: e.tensor_tensor(out=gates[:], in0=ex[:], in1=bc_last(vk[:, 6, :], E), op=ALU.mult), r=['ex', 'rden'], w=['gates'])
            for t in range(NT):
                bank, bkey = banks[t % 2]
                P.add('pe', lambda e, t=t, bank=bank: e.matmul(bank[:, 0:E], Ut, mask[:, t, :], start=True, stop=(t == 0)), r=['mask', 'tabs'], w=[bkey])
                for t2 in range(t):
                    P.add('pe', lambda e, t2=t2, t=t, bank=bank: e.matmul(bank[:, 0:E], ONt, mask[:, t2, :], start=False, stop=(t2 == t - 1)), r=['mask', 'tabs'], w=[bkey])
                P.add('act', lambda e, t=t, bank=bank: e.activation(out=rank[:, t, :], in_=bank[:, 0:E], func=AF.Copy), r=[bkey], w=['rank'])
            ioE = bc_mid(tabs[:, o_ie:o_ie + E], NT)
            for k in range(TOPK):
                P.add('dve', lambda e, k=k: e.tensor_tensor(out=t3[:], in0=oh[k][:], in1=gates[:], op=ALU.mult), r=[('oh', k), 'gates', 't3'], w=['t3'])
                P.add('dve', lambda e, k=k: e.tensor_reduce(out=wk[:, :, k], in_=t3[:], axis=AX.X, op=ALU.add), r=['t3'], w=['wk'])
                P.add('dve', lambda e, k=k: e.tensor_tensor(out=t3[:], in0=oh[k][:], in1=ioE, op=ALU.mult), r=[('oh', k), 'tabs', 'wk'], w=['t3'])
                P.add('dve', lambda e, k=k: e.tensor_reduce(out=vk[:, 7, :], in_=t3[:], axis=AX.X, op=ALU.add), r=['t3'], w=['ek'])
                P.add('dve', lambda e, k=k: e.tensor_tensor(out=t3[:], in0=oh[k][:], in1=rank[:], op=ALU.mult), r=[('oh', k), 'rank', 'ek'], w=['t3'])
                P.add('dve', lambda e, k=k: e.tensor_reduce(out=gf[:, :, k], in_=t3[:], axis=AX.X, op=ALU.add), r=['t3'], w=['gf'])
                P.add('dve', lambda e, k=k: e.tensor_scalar(out=t4[:], in0=gf[:, :, k], scalar1=float(CAP) - 0.5, scalar2=None, op0=ALU.is_lt), r=['gf'], w=['t4'])
                P.add('dve', lambda e, k=k: e.tensor_tensor(out=wk[:, :, k], in0=wk[:, :, k], in1=t4[:], op=ALU.mult), r=['t4', 'wk'], w=['wk'])
                P.add('dve', lambda e, k=k: e.tensor_scalar(out=gf[:, :, k], in0=gf[:, :, k], scalar1=float(CAP - 1), scalar2=None, op0=ALU.min), r=['gf', 't4'], w=['gf'])
                P.add('dve', lambda e, k=k: e.scalar_tensor_tensor(out=gf[:, :, k], in0=vk[:, 7, :], scalar=float(CAP), in1=gf[:, :, k], op0=ALU.mult, op1=ALU.add), r=['ek', 'gf'], w=['gf'])
            P.add('dve', lambda e: e.tensor_copy(out=gidx_i[:], in_=gf[:].rearrange("p t k -> p (t k)")), r=['gf'], w=['gidx_i'])
            ioc = tabs[:, o_ic:o_ic + CAP]
            for ee in range(E):
                pm, pmk = Pm[ee % 2], ('Pm', ee % 2)
                for t in range(NT):
                    P.add('dve', lambda e, ee=ee, t=t, pm=pm: e.tensor_scalar(out=pm[:, t, :], in0=ioc, scalar1=rank[:, t, ee:ee + 1], scalar2=mask[:, t, ee:ee + 1],
                                                                       op0=ALU.is_equal, op1=ALU.mult), r=['rank', 'mask', 'tabs'], w=[(pmk, t)])
                for s_ in range(NS):
                    col = ee * NS + s_
                    bank, bkey = banks[2 + (col // 4) % 4]
                    for t in range(NT):
                        P.add('pe', lambda e, t=t, s_=s_, pm=pm, bank=bank, col=col: e.matmul(bank[:, (col % 4) * 2:(col % 4) * 2 + 2], pm[:, t, s_ * 128:(s_ + 1) * 128],
                                                                                      tabs[:, o_tk + t:o_tk + t + 1].to_broadcast([128, 2]), start=(t == 0), stop=(t == NT - 1)),
                              r=[(pmk, t), 'tabs'], w=[bkey])
                    P.add('act', lambda e, col=col, bank=bank: e.activation(out=idxf[:, col:col + 1], in_=bank[:, (col % 4) * 2:(col % 4) * 2 + 1], func=AF.Copy), r=[bkey], w=['idxf'])
            P.add('dve', lambda e: e.tensor_copy(out=idx_i[:], in_=idxf[:]), r=['idxf'], w=['idx_i'])
            if debug:
                P.add('sp', lambda e: e.dma_start(out=dbg['gidx'], in_=gidx_i[:]), r=['gidx_i'], w=['dbggidx'], dma=True)
                P.add('sp', lambda e: e.dma_start(out=dbg['wk'], in_=wk[:].rearrange("p t k -> p (t k)")), r=['wk'], w=['dbgwk'], dma=True)
                P.add('sp', lambda e: e.dma_start(out=dbg['idx'], in_=idx_i[:]), r=['idx_i'], w=['dbgidx'], dma=True)
            P.emit()

        with ExitStack() as es:
            P = Prog(nc, st)
            KF = F // 128
            KHW = max(1, KD // 4)
            NW = 6
            NR = 2
            wbuf = [sb(es, "wbuf%d" % i, [128, KHW * 2 * GW]) for i in range(NW)]
            wraw = [sb(es, "wraw%d" % i, [128, KHW * 2 * GW]) for i in range(NR)]
            bbuf = [sb(es, "bbuf%d" % i, [1, 512]) for i in range(4)]
            Xg = [sb(es, "Xg%d" % i, [128, D]) for i in range(1)]
            XT = sb(es, "XT", [128, KD, CAP])
            actT = sb(es, "actT", [128, KF, CAP])
            actp = [sb(es, "actp%d" % i, [128, GW]) for i in range(4)]
            ystg = [sb(es, "ystg%d" % i, [128, 512]) for i in range(2)]
            eg = [sb(es, "eg%d" % i, [128, GW]) for i in range(2)]
            esg = [sb(es, "esg%d" % i, [128, GW]) for i in range(2)]
            eu = [sb(es, "eu%d" % i, [128, GW]) for i in range(2)]
            banks = [(psb(es, "p6b%d" % i), ('p6b', i)) for i in range(8)]
            accs = banks[0:4]
            tbs = banks[4:8]
            wcnt, bcnt, acnt, tcnt, ycnt, ecnt = [0], [0], [0], [0], [0], [0]
            rcnt = [0]

            def round_w(wb, wbk, rw, rwk):
                i = rcnt[0]
                rcnt[0] += 1
                if i % 2 == 0:
                    P.add('dve', lambda e, wb=wb, rw=rw: e.tensor_copy(out=r32(wb[:]), in_=rw[:]), r=[(rwk, 0), (rwk, 1)], w=[(wbk, 0), (wbk, 1)])
                else:
                    P.add('act', lambda e, wb=wb, rw=rw: e.activation(out=r32(wb[:]), in_=rw[:], func=AF.Copy), r=[(rwk, 0), (rwk, 1)], w=[(wbk, 0), (wbk, 1)])
            NCG = F // GW
            NKP = KD // KHW
            DB = 512 if D >= 512 else D
            KHD = (KHW * 2 * GW) // DB
            for ee in range(E):
                for s_ in range(NS):
                    xg, xgk = Xg[0], ('Xg', 0)
                    col = ee * NS + s_
                    P.add('pool', lambda e, xg=xg, col=col: e.indirect_dma_start(out=xg[:], out_offset=None, in_=H2, in_offset=bass.IndirectOffsetOnAxis(ap=idx_i[:, col:col + 1], axis=0)),
                          r=['idx_i'], w=[xgk], dma=True)
                    for k4 in range(0, KD, 4):
                        bank, bkey = tbs[tcnt[0] % 4]
                        tcnt[0] += 1
                        nk = min(4, KD - k4)
                        for kk in range(nk):
                            P.add('pe', lambda e, k=k4 + kk, kk=kk, bank=bank, xg=xg: e.transpose(bank[:, kk * 128:(kk + 1) * 128], xg[:, k * 128:(k + 1) * 128], ident), r=[xgk, 'tabs'], w=[bkey])
                        P.add('act', lambda e, bank=bank, k4=k4, nk=nk, s_=s_: e.activation(out=r32(XT[:, k4:k4 + nk, s_ * 128:(s_ + 1) * 128]), in_=bank[:, 0:nk * 128].rearrange("p (k t) -> p k t", k=nk), func=AF.Copy),
                              r=[bkey], w=[('XT', s_)])
                pendT = []
                for cg in range(NCG):
                    bb, bbk = bbuf[bcnt[0] % 4], ('bbuf', bcnt[0] % 4)
                    bcnt[0] += 1
                    P.add('pool', lambda e, bb=bb, ee=ee, cg=cg: e.dma_start(out=r32(bb[0:1, 0:GW]), in_=b_gu[ee:ee + 1, cg * GW:(cg + 1) * GW]), w=[(bbk, 0)], dma=True)
                    P.add('pool', lambda e, bb=bb, ee=ee, cg=cg: e.dma_start(out=r32(bb[0:1, GW:2 * GW]), in_=b_gu[ee:ee + 1, F + cg * GW:F + (cg + 1) * GW]), w=[(bbk, 1)], dma=True)
                    pieces = []
                    for kp in range(NKP):
                        wb, wbk = wbuf[wcnt[0] % NW], ('wbuf', wcnt[0] % NW)
                        wcnt[0] += 1
                        wv = wb[:].rearrange("p (k two n) -> p k two n", k=KHW, two=2)
                        rw, rwk = wraw[rcnt[0] % NR], ('wraw', rcnt[0] % NR)
                        rv = rw[:].rearrange("p (k two n) -> p k two n", k=KHW, two=2)
                        for gu in range(2):
                            src = w_gu[ee, kp * KHW * 128:(kp + 1) * KHW * 128, gu * F + cg * GW:gu * F + (cg + 1) * GW].rearrange("(k p) n -> p k n", p=128)
                            P.add('sp', lambda e, rv=rv, gu=gu, src=src: e.dma_start(out=rv[:, :, gu, :], in_=src), w=[(rwk, gu)], dma=True)
                        round_w(wb, wbk, rw, rwk)
                        pieces.append((wv, wbk))
                    for s_ in range(NS):
                        bank, bkey = accs[acnt[0] % 4]
                        acnt[0] += 1
                        for k in range(KD):
                            wv, wbk = pieces[k // KHW]
                            kk = k % KHW
                            P.add('pe', lambda e, k=k, kk=kk, s_=s_, bank=bank, wv=wv: e.matmul(bank[:, 0:2 * GW], r32(XT[:, k, s_ * 128:(s_ + 1) * 128]),
                                                                                           r32(wv[:, kk, :, :].rearrange("p two n -> p (two n)")), start=(k == 0), stop=False),
                                  r=[('XT', s_), (wbk, 0), (wbk, 1)], w=[bkey])
                        P.add('pe', lambda e, bank=bank, bb=bb: e.matmul(bank[:, 0:2 * GW], r32(ones1r[0:1, :]), r32(bb[0:1, 0:2 * GW]), start=False, stop=True), r=['ones1r', (bbk, 0), (bbk, 1)], w=[bkey])
                        i2 = ecnt[0] % 2
                        ecnt[0] += 1
                        ap_, apk = actp[acnt[0] % 4], ('actp', acnt[0] % 4)
                        P.add('dve', lambda e, bank=bank, i2=i2: e.tensor_scalar(out=eg[i2][:], in0=bank[:, 0:GW], scalar1=7.0, scalar2=None, op0=ALU.min), r=[bkey], w=[('eg', i2)])
                        P.add('act', lambda e, i2=i2: e.activation(out=esg[i2][:], in_=eg[i2][:], func=AF.Sigmoid, scale=1.702), r=[('eg', i2)], w=[('esg', i2)])
                        P.add('dve', lambda e, bank=bank, i2=i2: e.tensor_scalar(out=eu[i2][:], in0=bank[:, GW:2 * GW], scalar1=-7.0, scalar2=7.0, op0=ALU.max, op1=ALU.min), r=[bkey], w=[('eu', i2)])
                        P.add('pool', lambda e, i2=i2: e.tensor_tensor(out=eg[i2][:], in0=eg[i2][:], in1=esg[i2][:], op=ALU.mult), r=[('eg', i2), ('esg', i2)], w=[('eg', i2)])
                        P.add('dve', lambda e, i2=i2, ap_=ap_: e.scalar_tensor_tensor(out=ap_[:], in0=eu[i2][:], scalar=1.0, in1=eg[i2][:], op0=ALU.add, op1=ALU.mult),
                              r=[('eu', i2), ('eg', i2)], w=[apk])
                        def emit_T(ap_=ap_, apk=apk, cg=cg, s_=s_):
                            tb, tbk = tbs[tcnt[0] % 4]
                            tcnt[0] += 1
                            nck = GW // 128
                            for kk in range(nck):
                                P.add('pe', lambda e, kk=kk, tb=tb, ap_=ap_: e.transpose(tb[:, kk * 128:(kk + 1) * 128], ap_[:, kk * 128:(kk + 1) * 128], ident), r=[apk, 'tabs'], w=[tbk])
                            P.add('act', lambda e, tb=tb, cg=cg, s_=s_, nck=nck: e.activation(out=r32(actT[:, cg * nck:(cg + 1) * nck, s_ * 128:(s_ + 1) * 128]),
                                                                                         in_=tb[:, 0:nck * 128].rearrange("p (k t) -> p k t", k=nck), func=AF.Copy),
                                  r=[tbk], w=[('actT', s_)])
                        if pendT:
                            pendT.pop(0)()
                        pendT.append(emit_T)
                while pendT:
                    pendT.pop(0)()
                for db in range(D // DB):
                    bb, bbk = bbuf[bcnt[0] % 4], ('bbuf', bcnt[0] % 4)
                    bcnt[0] += 1
                    P.add('pool', lambda e, bb=bb, ee=ee, db=db: e.dma_start(out=r32(bb[0:1, 0:DB]), in_=b_dn[ee:ee + 1, db * DB:(db + 1) * DB]), w=[(bbk, 0), (bbk, 1)], dma=True)
                    pieces = []
                    for kp in range(KF // KHD):
                        wb, wbk = wbuf[wcnt[0] % NW], ('wbuf', wcnt[0] % NW)
                        wcnt[0] += 1
                        wv = wb[:, 0:KHD * DB].rearrange("p (k n) -> p k n", k=KHD)
                        src = w_dn[ee, kp * KHD * 128:(kp + 1) * KHD * 128, db * DB:(db + 1) * DB].rearrange("(k p) n -> p k n", p=128)
                        rw, rwk = wraw[rcnt[0] % NR], ('wraw', rcnt[0] % NR)
                        rv = rw[:, 0:KHD * DB].rearrange("p (k n) -> p k n", k=KHD)
                        P.add('sp', lambda e, rv=rv, src=src: e.dma_start(out=rv, in_=src), w=[(rwk, 0), (rwk, 1)], dma=True)
                        round_w(wb, wbk, rw, rwk)
                        pieces.append((wv, wbk))
                    for s_ in range(NS):
                        bank, bkey = accs[acnt[0] % 4]
                        acnt[0] += 1
                        for k in range(KF):
                            wv, wbk = pieces[k // KHD]
                            kk = k % KHD
                            P.add('pe', lambda e, k=k, kk=kk, s_=s_, bank=bank, wv=wv: e.matmul(bank[:, 0:DB], r32(actT[:, k, s_ * 128:(s_ + 1) * 128]), r32(wv[:, kk, :]), start=(k == 0), stop=False),
                                  r=[('actT', s_), (wbk, 0), (wbk, 1)], w=[bkey])
                        P.add('pe', lambda e, bank=bank, bb=bb: e.matmul(bank[:, 0:DB], r32(ones1r[0:1, :]), r32(bb[0:1, 0:DB]), start=False, stop=True), r=['ones1r', (bbk, 0), (bbk, 1)], w=[bkey])
                        yb, ybk = ystg[ycnt[0] % 2], ('ystg', ycnt[0] % 2)
                        if ycnt[0] % 2 == 0:
                            P.add('act', lambda e, bank=bank, yb=yb: e.activation(out=yb[:, 0:DB], in_=bank[:, 0:DB], func=AF.Copy), r=[bkey], w=[ybk])
                        else:
                            P.add('dve', lambda e, bank=bank, yb=yb: e.tensor_copy(out=yb[:, 0:DB], in_=bank[:, 0:DB]), r=[bkey], w=[ybk])
                        ycnt[0] += 1
                        r0 = (ee * NS + s_) * 128
                        P.add('sp', lambda e, yb=yb, r0=r0, db=db: e.dma_start(out=YS[r0:r0 + 128, db * DB:(db + 1) * DB], in_=yb[:, 0:DB]), r=[ybk], w=['YS'], dma=True)
            if debug:
                P.add('sp', lambda e: e.dma_start(out=dbg['ys'], in_=YS), r=['YS'], w=['dbgys'], dma=True)
            P.emit()

        with ExitStack() as es:
            P = Prog(nc, st)
            Yg = [sb(es, "Yg%d" % i, [128, D]) for i in range(4)]
            acc = [sb(es, "acc%d" % i, [128, D]) for i in range(2)]
            x1t = [sb(es, "x1t%d" % i, [128, D]) for i in range(2)]
            g2t = sb(es, "g2t", [128, D])
            fgb = sb(es, "fgb", [128, D])
            ss7 = [sb(es, "ss7%d" % i, [128, 4]) for i in range(2)]
            P.add('act', lambda e: e.dma_start(out=fgb[:], in_=final_g[0, :].partition_broadcast(128)), w=['fgb'], dma=True)
            cur_ty = -1
            gc = 0
            for t in range(NT):
                ty = 0 if t < TP else 1
                if ty != cur_ty:
                    load_mod(P, g2t, 'g2t', 5, ty)
                    cur_ty = ty
                i2 = t % 2
                ac, ack = acc[i2], ('acc', i2)
                xt, xtk = x1t[i2], ('x1t', i2)
                ss, sk = ss7[i2], ('ss7', i2)
                P.add('sp', lambda e, t=t, xt=xt: e.dma_start(out=xt[:], in_=X1[t * 128:(t + 1) * 128, :]), w=[xtk], dma=True)
                for k in range(TOPK):
                    yg, ygk = Yg[gc % 4], ('Yg', gc % 4)
                    gc += 1
                    c_ = t * TOPK + k
                    P.add('pool', lambda e, yg=yg, c_=c_: e.indirect_dma_start(out=yg[:], out_offset=None, in_=YS, in_offset=bass.IndirectOffsetOnAxis(ap=gidx_i[:, c_:c_ + 1], axis=0)),
                          r=['gidx_i'], w=[ygk], dma=True)
                    if k == 0:
                        P.add('dve', lambda e, yg=yg, ac=ac, t=t, k=k: e.tensor_scalar(out=ac[:], in0=yg[:], scalar1=wk[:, t, k:k + 1], scalar2=None, op0=ALU.mult), r=[ygk, 'wk'], w=[ack])
                    else:
                        P.add('dve', lambda e, yg=yg, ac=ac, t=t, k=k: e.scalar_tensor_tensor(out=ac[:], in0=yg[:], scalar=wk[:, t, k:k + 1], in1=ac[:], op0=ALU.mult, op1=ALU.add), r=[ygk, 'wk', ack], w=[ack])
                P.add('pool', lambda e, ac=ac: e.tensor_tensor(out=ac[:], in0=ac[:], in1=g2t[:], op=ALU.mult), r=[ack, 'g2t'], w=[ack])
                P.add('dve', lambda e, ac=ac, xt=xt: e.tensor_tensor(out=xt[:], in0=xt[:], in1=ac[:], op=ALU.add), r=[ack, xtk], w=[xtk])
                P.add('act', lambda e, ac=ac, xt=xt, ss=ss: e.activation(out=ac[:], in_=xt[:], func=AF.Square, accum_out=ss[:, 0:1]), r=[xtk, ack], w=[ack, sk])
                P.add('dve', lambda e, ss=ss: e.tensor_scalar(out=ss[:, 1:2], in0=ss[:, 0:1], scalar1=1.0 / D, scalar2=NORM_EPS, op0=ALU.mult, op1=ALU.add), r=[sk], w=[sk])
                P.add('act', lambda e, ss=ss: e.sqrt(ss[:, 3:4], ss[:, 1:2]), r=[sk], w=[sk])
                P.add('dve', lambda e, ss=ss: e.reciprocal(out=ss[:, 2:3], in_=ss[:, 3:4]), r=[sk], w=[sk])
                P.add('dve', lambda e, ac=ac, xt=xt, ss=ss: e.scalar_tensor_tensor(out=ac[:], in0=xt[:], scalar=ss[:, 2:3], in1=fgb[:], op0=ALU.mult, op1=ALU.mult), r=[xtk, sk, 'fgb', ack], w=[ack])
                P.add('sp', lambda e, t=t, ac=ac: e.dma_start(out=y[t * 128:(t + 1) * 128, :], in_=ac[:]), r=[ack], w=[('y', t)], dma=True)
            P.emit(final=True)
    return nc


def run_cfg(cfg, inputs, debug=False):
    D, TP, NT, H, CW = cfg.D, cfg.TP, cfg.NT, cfg.H, cfg.CW
    f = lambda a: np.ascontiguousarray(np.asarray(a, dtype=np.float32))
    x_prompt, x_sample = f(inputs['x_prompt']), f(inputs['x_sample'])
    state_ret, state_conv = f(inputs['state_ret'])[0], f(inputs['state_conv'])[0]
    c_prompt, c_sample = f(inputs['c_prompt']), f(inputs['c_sample'])
    B = x_prompt.shape[0]
    HALF = TP * 128
    shared = {
        'w_ada': f(inputs['w_ada'])[0], 'b_ada': f(inputs['b_ada']).reshape(1, -1), 'norm1_g': f(inputs['norm1_g']).reshape(1, -1),
        'norm2_g': f(inputs['norm2_g']).reshape(1, -1), 'w_in': f(inputs['w_in'])[0], 'conv_w': f(inputs['conv_w']).reshape(1, -1),
        'ret_gn': f(inputs['ret_gn']).reshape(1, -1), 'w_o': f(inputs['w_o'])[0], 'w_router': f(inputs['w_router'])[0],
        'b_router': f(inputs['b_router']).reshape(1, -1), 'w_gu': f(inputs['w_gu'])[0], 'b_gu': f(inputs['b_gu'])[0],
        'w_dn': f(inputs['w_dn'])[0], 'b_dn': f(inputs['b_dn'])[0], 'final_g': f(inputs['final_g']).reshape(1, -1),
    }
    in_maps = []
    for c in range(NCORES):
        b, half = c // 2, c % 2
        s0 = c * SEQ_PER_CORE
        xm = np.concatenate([x_prompt[b, half * HALF:(half + 1) * HALF], x_sample[s0:s0 + SEQ_PER_CORE].reshape(128, D)], axis=0)
        c32 = np.zeros((32, D), np.float32)
        c32[0] = c_prompt[b]
        c32[1:17] = c_sample[s0:s0 + SEQ_PER_CORE]
        m = dict(shared)
        m.update({'xm': xm, 'xp': np.ascontiguousarray(x_prompt[b, 0:HALF]), 'c32': c32,
                  'sret': np.ascontiguousarray(state_ret[s0:s0 + SEQ_PER_CORE]),
                  'sconv': np.ascontiguousarray(state_conv[s0:s0 + SEQ_PER_CORE].reshape(32, CW)),
                  'tab': make_tables(cfg, half)})
        in_maps.append(m)
    nc = build_program(cfg, debug=debug)
    res = run_bass_kernel_spmd(nc, in_maps, core_ids=list(range(NCORES)))
    R = res.results
    y_prompt = np.zeros((B, 2 * HALF, D), np.float32)
    y_sample = np.zeros((NCORES * SEQ_PER_CORE, DEC_SEQ, D), np.float32)
    ret_p = np.zeros((1, B, H, 128, 128), np.float32)
    conv_p = np.zeros((1, B, 2, CW), np.float32)
    ret_s = np.zeros((1, NCORES * SEQ_PER_CORE, H, 128, 128), np.float32)
    conv_s = np.zeros((1, NCORES * SEQ_PER_CORE, 2, CW), np.float32)
    for c in range(NCORES):
        b, half = c // 2, c % 2
        s0 = c * SEQ_PER_CORE
        yy = R[c]['y']
        y_prompt[b, half * HALF:(half + 1) * HALF] = yy[0:HALF]
        y_sample[s0:s0 + SEQ_PER_CORE] = yy[HALF:].reshape(SEQ_PER_CORE, DEC_SEQ, D)
        if half == 1:
            ret_p[0, b] = R[c]['ret_p']
            conv_p[0, b] = R[c]['conv_p']
        ret_s[0, s0:s0 + SEQ_PER_CORE] = R[c]['ret_s']
        conv_s[0, s0:s0 + SEQ_PER_CORE] = R[c]['conv_s'].reshape(SEQ_PER_CORE, DEC_SEQ, CW)[:, DEC_SEQ - 2:DEC_SEQ, :]
    outs = (y_prompt, y_sample, ret_p, conv_p, ret_s, conv_s)
    if debug:
        return outs, R
    return outs


def kernel(**inputs):
    cfg = Cfg(D=2048, E=32, TP=8)
    return run_cfg(cfg, inputs)
```

```python
from contextlib import ExitStack
import numpy as np
import concourse.bass as bass
import concourse.mybir as mybir
from concourse.bass_utils import run_bass_kernel_spmd

F32 = mybir.dt.float32
F32R = mybir.dt.float32r
I32 = mybir.dt.int32
ALU = mybir.AluOpType
AF = mybir.ActivationFunctionType
AX = mybir.AxisListType

NCORES = 8
DEC_SEQ = 8
SEQ_PER_CORE = 16
ROPE_BASE = 10000.0
NORM_EPS = 1e-6
PAST_LEN = 16384
TOPK = 4
NRING = 24
NBW = 10


NS_DEFAULT = 3


class Cfg:
    def __init__(s, D=2048, E=32, TP=8):
        s.D = D
        s.KD = D // 128
        s.RW = D // 2
        s.CW = D // 2
        s.H = s.RW // 128
        s.IN = 4 * s.RW + 3 * s.CW
        s.E = E
        s.F = D
        s.TP = TP
        s.NPRE = TP
        s.NT = TP + 1
        s.CB = 256
        s.CAP = 128 * NS_DEFAULT
        s.NS = s.CAP // 128
        s.GW = 256
        s.KH = max(1, s.KD // 2)
        s.NTOK = s.NT * 128


class SyncState:
    def __init__(s, nc, es):
        s.engs = ('pe', 'act', 'dve', 'pool', 'sp')
        s.sem = {e: es.enter_context(nc.semaphore("sem_" + e)) for e in s.engs}
        s.ring = [es.enter_context(nc.semaphore("ring%d" % i)) for i in range(NRING + NBW)]
        s.count = {e: 0 for e in s.engs}
        s.ringval = [0] * (NRING + NBW)
        s.ndma = 0


class Prog:
    def __init__(s, nc, st):
        s.nc = nc
        s.st = st
        s.ops = []

    def add(s, eng, fn, r=(), w=(), dma=False, ring=None):
        s.ops.append((eng, fn, tuple(r), tuple(w), dma, ring))

    def emit(s, final=False):
        nc, st, ops = s.nc, s.st, s.ops
        n = len(ops)
        deps = [None] * n
        last_w, readers = {}, {}
        for i, (eng, fn, r, w, dma, _rg) in enumerate(ops):
            d = set()
            for k in r:
                if k in last_w:
                    d.add(last_w[k])
            for k in w:
                if k in last_w:
                    d.add(last_w[k])
                d.update(readers.get(k, ()))
            d.discard(i)
            deps[i] = d
            for k in r:
                readers.setdefault(k, []).append(i)
            for k in w:
                last_w[k] = i
                readers[k] = []
        signal = [False] * n
        for i in range(n):
            ei = ops[i][0]
            for d in deps[i]:
                if ops[d][4]:
                    continue
                if ops[d][0] == ei and ei == 'pe':
                    continue
                signal[d] = True
        lastop = {}
        for i in range(n):
            if not ops[i][4]:
                lastop[ops[i][0]] = i
        for e, i in lastop.items():
            signal[i] = True
        start_count = dict(st.count)
        start_ring = list(st.ringval)
        tok = [None] * n
        for i in range(n):
            eng, fn, r, w, dma, rg = ops[i]
            if dma:
                if rg is None:
                    g = st.ndma
                    st.ndma += 1
                    ri = g % NRING
                else:
                    ri = NRING + rg
                st.ringval[ri] += 16
                tok[i] = (('ring', ri), st.ringval[ri])
            elif signal[i]:
                st.count[eng] += 1
                tok[i] = (('eng', eng), st.count[eng])
        end_count = dict(st.count)
        end_ring = list(st.ringval)

        def semof(key):
            return st.sem[key[1]] if key[0] == 'eng' else st.ring[key[1]]

        def run_engine(me, eobj):
            waited = {}

            def wait(key, val):
                if val <= 0 or waited.get(key, 0) >= val:
                    return
                eobj.wait_ge(semof(key), val)
                waited[key] = val
            for e in st.engs:
                wait(('eng', e), start_count[e])
            for ri in range(NRING + NBW):
                wait(('ring', ri), start_ring[ri])
            for i in range(n):
                eng, fn, r, w, dma, _rg = ops[i]
                if eng != me:
                    continue
                for d in sorted(deps[i]):
                    if ops[d][0] == me and me == 'pe' and not ops[d][4]:
                        continue
                    key, val = tok[d]
                    wait(key, val)
                ins = fn(eobj)
                if dma:
                    ins.then_inc(semof(tok[i][0]), 16)
                elif signal[i]:
                    ins.then_inc(st.sem[me], 1)
            if final and me == 'sp':
                for e in st.engs:
                    wait(('eng', e), end_count[e])
                for ri in range(NRING + NBW):
                    wait(('ring', ri), end_ring[ri])

        with nc.Block() as block:
            @block.tensor
            def _(e):
                run_engine('pe', e)

            @block.scalar
            def _(e):
                run_engine('act', e)

            @block.vector
            def _(e):
                run_engine('dve', e)

            @block.gpsimd
            def _(e):
                run_engine('pool', e)

            @block.sync
            def _(e):
                run_engine('sp', e)


def table_layout(cfg):
    H, NT, NPRE, E = cfg.H, cfg.NT, cfg.NPRE, cfg.E
    items = [('ident', 128), ('ropec', NT * 64), ('ropes', NT * 64), ('prec', NPRE * 64), ('pres', NPRE * 64),
             ('dec', 2 * 2 * H), ('kdecpre', NPRE * H), ('gL', 2 * H), ('maskT', 2 * 128), ('convm', 3 * 4 * 128),
             ('seqmask', 16), ('U', 128), ('ONES', 128), ('iotac', cfg.CAP), ('iotaE', E), ('tokid', NT)]
    off, o = {}, 0
    for k, sz in items:
        off[k] = (o, sz)
        o += sz
    return off, o


def make_tables(cfg, half):
    H, NT, NPRE, E, TP = cfg.H, cfg.NT, cfg.NPRE, cfg.E, cfg.TP
    off, tot = table_layout(cfg)
    T = np.zeros((128, tot), np.float32)

    def put(name, arr):
        o, sz = off[name]
        T[:, o:o + sz] = np.asarray(arr, np.float32).reshape(128, sz)
    p = np.arange(128)
    put('ident', np.eye(128))
    inv = (np.float32(ROPE_BASE) ** (-np.arange(64, dtype=np.float32) / np.float32(64))).astype(np.float32)
    pos = np.zeros((128, NT), np.float32)
    for t in range(TP):
        pos[:, t] = half * TP * 128 + t * 128 + p
    pos[:, TP] = PAST_LEN + (p % DEC_SEQ)
    ang = (pos[:, :, None] * inv[None, None, :]).astype(np.float32).astype(np.float64)
    put('ropec', np.cos(ang))
    put('ropes', np.sin(ang))
    posp = (np.arange(NPRE)[None, :] * 128 + p[:, None]).astype(np.float32)
    angp = (posp[:, :, None] * inv[None, None, :]).astype(np.float32).astype(np.float64)
    put('prec', np.cos(angp))
    put('pres', np.sin(angp))
    hh = np.arange(H, dtype=np.float64)
    logg = np.log1p(-np.exp2(-5.0 - hh))
    sc = 128.0 ** -0.5
    dec = np.zeros((128, 2, 2 * H))
    i0 = p.astype(np.float64)
    i1 = (p % DEC_SEQ).astype(np.float64)
    for ty, ii in ((0, i0), (1, i1)):
        dec[:, ty, :H] = np.exp(logg[None, :] * (ii[:, None] + 1.0))
        dec[:, ty, H:] = np.exp(-logg[None, :] * (ii[:, None] + 1.0)) * sc
    put('dec', dec)
    L = NPRE * 128
    j = (np.arange(NPRE)[None, :] * 128 + p[:, None]).astype(np.float64)
    kd = np.exp(logg[None, None, :] * (L - 1.0 - j)[:, :, None]) * sc * (1.0 if half == 1 else 0.0)
    put('kdecpre', kd)
    gL = np.zeros((128, 2, H))
    gL[:, 0, :] = np.exp(logg * 128.0)[None, :]
    gL[:, 1, :] = np.exp(logg * float(DEC_SEQ))[None, :]
    put('gL', gL)
    jj, ii = p[:, None], p[None, :]
    m = np.zeros((128, 2, 128))
    m[:, 0, :] = (jj <= ii)
    m[:, 1, :] = (jj <= ii) & (jj // DEC_SEQ == ii // DEC_SEQ)
    put('maskT', m)
    cm = np.zeros((128, 3, 4, 128))
    flag = 1.0 if half == 1 else 0.0
    for ty in range(2):
        cm[:, ty, 0, :] = (jj == ii - 1)
        cm[:, ty, 1, :] = (jj == ii - 2)
        f = flag if ty == 0 else 1.0
        cm[127, ty, 2, 0] = f
        cm[126, ty, 3, 0] = f
        cm[127, ty, 3, 1] = f
    same = (jj // DEC_SEQ == ii // DEC_SEQ)
    cm[:, 2, 0, :] = (jj == ii - 1) & same
    cm[:, 2, 1, :] = (jj == ii - 2) & same
    for b in range(SEQ_PER_CORE):
        cm[2 * b + 1, 2, 2, 8 * b + 0] = 1.0
        cm[2 * b + 0, 2, 3, 8 * b + 0] = 1.0
        cm[2 * b + 1, 2, 3, 8 * b + 1] = 1.0
    put('convm', cm)
    put('seqmask', (p[:, None] // DEC_SEQ == np.arange(16)[None, :]))
    put('U', (jj < ii))
    put('ONES', np.ones((128, 128)))
    put('iotac', np.tile(np.arange(cfg.CAP)[None, :], (128, 1)))
    put('iotaE', np.tile(np.arange(E)[None, :], (128, 1)))
    put('tokid', np.arange(NT)[None, :] * 128 + p[:, None])
    return T


def build_program(cfg, debug=False):
    D, KD, RW, CW, H, IN, E, F = cfg.D, cfg.KD, cfg.RW, cfg.CW, cfg.H, cfg.IN, cfg.E, cfg.F
    TP, NPRE, NT, CB, CAP, NS, GW, KH, NTOK = cfg.TP, cfg.NPRE, cfg.NT, cfg.CB, cfg.CAP, cfg.NS, cfg.GW, cfg.KH, cfg.NTOK
    toff, ttot = table_layout(cfg)
    nc = bass.Bass("TRN2", target_bir_lowering=False)

    def din(name, shape, dt=F32):
        return nc.dram_tensor(name, list(shape), dt, kind="ExternalInput").ap()

    def dout(name, shape, dt=F32):
        return nc.dram_tensor(name, list(shape), dt, kind="ExternalOutput").ap()

    def dscr(name, shape, dt=F32):
        return nc.dram_tensor(name, list(shape), dt, kind="Internal").ap()

    xm = din("xm", [NTOK, D])
    xp = din("xp", [NPRE * 128, D])
    c32 = din("c32", [32, D])
    sret = din("sret", [SEQ_PER_CORE, H, 128, 128])
    sconv = din("sconv", [32, CW])
    tab = din("tab", [128, ttot])
    w_ada = din("w_ada", [D, 6 * D])
    b_ada = din("b_ada", [1, 6 * D])
    norm1_g = din("norm1_g", [1, D])
    norm2_g = din("norm2_g", [1, D])
    w_in = din("w_in", [D, IN])
    conv_w = din("conv_w", [1, 3 * CW])
    ret_gn = din("ret_gn", [1, RW])
    w_o = din("w_o", [D, D])
    w_router = din("w_router", [D, E])
    b_router = din("b_router", [1, E])
    w_gu = din("w_gu", [E, D, 2 * F])
    b_gu = din("b_gu", [E, 2 * F])
    w_dn = din("w_dn", [E, F, D])
    b_dn = din("b_dn", [E, D])
    final_g = din("final_g", [1, D])

    y = dout("y", [NTOK, D])
    ret_p = dout("ret_p", [H, 128, 128])
    conv_p = dout("conv_p", [2, CW])
    ret_s = dout("ret_s", [SEQ_PER_CORE, H, 128, 128])
    conv_s = dout("conv_s", [128, CW])

    MODD = dscr("MODD", [32, 6 * D])
    PROJ = dscr("PROJ", [NTOK, IN])
    MIX = dscr("MIX", [NTOK, D])
    X1 = dscr("X1", [NTOK, D])
    H2 = dscr("H2", [NTOK, D])
    YS = dscr("YS", [E * CAP, D])
    dbg = {}
    if debug:
        dbg['modd'] = dout("dbg_modd", [32, 6 * D])
        dbg['proj'] = dout("dbg_proj", [NTOK, IN])
        dbg['mix'] = dout("dbg_mix", [NTOK, D])
        dbg['x1'] = dout("dbg_x1", [NTOK, D])
        dbg['h2'] = dout("dbg_h2", [NTOK, D])
        dbg['logits'] = dout("dbg_logits", [128, NT * E])
        dbg['spre'] = dout("dbg_spre", [128, H * 128])
        dbg['gidx'] = dout("dbg_gidx", [128, NT * TOPK], I32)
        dbg['wk'] = dout("dbg_wk", [128, NT * TOPK])
        dbg['idx'] = dout("dbg_idx", [128, E * NS], I32)
        dbg['ys'] = dout("dbg_ys", [E * CAP, D])

    with ExitStack() as top:
        st = SyncState(nc, top)

        def sb(es, name, shape, dt=F32):
            return es.enter_context(nc.sbuf_tensor(name, list(shape), dt))

        def psb(es, name):
            return es.enter_context(nc.psum_tensor(name, [128, 512], F32))

        tabs = sb(top, "tabs", [128, ttot])
        S = sb(top, "S", [128, H, 128])
        zprev = sb(top, "zprev", [128, CW])
        logits = sb(top, "logits", [128, NT, E])
        idx_i = sb(top, "idx_i", [128, E * NS], I32)
        gidx_i = sb(top, "gidx_i", [128, NT * TOPK], I32)
        wk = sb(top, "wk", [128, NT, TOPK])
        ones1 = sb(top, "ones1", [1, 128])
        ones1r = sb(top, "ones1r", [1, 128])

        def tb(name):
            o, sz = toff[name]
            return tabs[:, o:o + sz]
        ident = tb('ident')

        def r32(ap):
            return ap.bitcast(F32R)

        def bc_mid(ap2, n):
            return ap2.unsqueeze(1).to_broadcast([ap2.shape[0], n, ap2.shape[1]])

        def bc_last(ap2, n):
            return ap2.unsqueeze(2).to_broadcast([ap2.shape[0], ap2.shape[1], n])

        with ExitStack() as es:
            P = Prog(nc, st)
            csb = sb(es, "csb", [32, D])
            cT = sb(es, "cT", [128, KD, 32])
            modsb = sb(es, "modsb", [32, 6 * D])
            wab = [sb(es, "wab%d" % i, [128, KD, CB]) for i in range(2)]
            bab = [sb(es, "bab%d" % i, [32, CB]) for i in range(2)]
            ng = sb(es, "ng", [32, 2, D])
            pst = psb(es, "p0t")
            pm = [psb(es, "p0m%d" % i) for i in range(2)]
            P.add('sp', lambda e: e.dma_start(out=tabs[:], in_=tab), w=['tabs'], dma=True)
            P.add('sp', lambda e: e.dma_start(out=csb[:], in_=c32), w=['csb'], dma=True)
            P.add('pool', lambda e: e.memset(ones1[:], 1.0), w=['ones1'])
            P.add('act', lambda e: e.activation(out=r32(ones1r[:]), in_=ones1[:], func=AF.Copy), r=['ones1'], w=['ones1r'])
            P.add('act', lambda e: e.activation(out=csb[:], in_=csb[:], func=AF.Silu), r=['csb'], w=['csb'])
            for k in range(KD):
                P.add('pe', lambda e, k=k: e.transpose(pst[:, k * 32:(k + 1) * 32], csb[:, k * 128:(k + 1) * 128], ident[0:32, 0:32]),
                      r=['csb', 'tabs'], w=['pst'])
            P.add('dve', lambda e: e.tensor_copy(out=r32(cT[:].rearrange("p k m -> p (k m)")), in_=pst[:, 0:KD * 32]), r=['pst'], w=['cT'])
            P.add('sp', lambda e: e.dma_start(out=ng[:, 0, :], in_=norm1_g[0, :].partition_broadcast(32)), w=['ng0'], dma=True)
            P.add('sp', lambda e: e.dma_start(out=ng[:, 1, :], in_=norm2_g[0, :].partition_broadcast(32)), w=['ng1'], dma=True)
            war = w_ada.rearrange("(k p) n -> p k n", p=128)
            NCB = 6 * D // CB
            for cb in range(NCB):
                wb, bb, pp = wab[cb % 2], bab[cb % 2], pm[cb % 2]
                P.add('pool', lambda e, wb=wb, cb=cb: e.dma_start(out=r32(wb[:]), in_=war[:, :, cb * CB:(cb + 1) * CB]), w=[('wab', cb % 2)], dma=True)
                P.add('act', lambda e, bb=bb, cb=cb: e.dma_start(out=bb[:], in_=b_ada[0, cb * CB:(cb + 1) * CB].partition_broadcast(32)),
                      w=[('bab', cb % 2)], dma=True)
                for k in range(KD):
                    P.add('pe', lambda e, k=k, wb=wb, pp=pp: e.matmul(pp[0:32, 0:CB], r32(cT[:, k, :]), r32(wb[:, k, :]), start=(k == 0), stop=(k == KD - 1)),
                          r=['cT', ('wab', cb % 2)], w=[('pm', cb % 2)])
                P.add('dve', lambda e, pp=pp, bb=bb, cb=cb: e.tensor_tensor(out=modsb[:, cb * CB:(cb + 1) * CB], in0=pp[0:32, 0:CB], in1=bb[:], op=ALU.add),
                      r=[('pm', cb % 2), ('bab', cb % 2)], w=[('mod', cb * CB // D)])
            P.add('dve', lambda e: e.scalar_tensor_tensor(out=modsb[:, D:2 * D], in0=modsb[:, D:2 * D], scalar=1.0, in1=ng[:, 0, :], op0=ALU.add, op1=ALU.mult),
                  r=[('mod', 1), 'ng0'], w=[('mod', 1)])
            P.add('dve', lambda e: e.scalar_tensor_tensor(out=modsb[:, 4 * D:5 * D], in0=modsb[:, 4 * D:5 * D], scalar=1.0, in1=ng[:, 1, :], op0=ALU.add, op1=ALU.mult),
                  r=[('mod', 4), 'ng1'], w=[('mod', 4)])
            P.add('sp', lambda e: e.dma_start(out=MODD, in_=modsb[:]), r=[('mod', i) for i in range(6)], w=['MODD'], dma=True)
            if debug:
                P.add('sp', lambda e: e.dma_start(out=dbg['modd'], in_=modsb[:]), r=[('mod', i) for i in range(6)], w=['dbgmodd'], dma=True)
            P.emit()

        def load_mod(P, tile_, key, sec, ty, q='act'):
            if ty == 0:
                P.add(q, lambda e: e.dma_start(out=tile_[:], in_=MODD[0, sec * D:(sec + 1) * D].partition_broadcast(128)), w=[key], dma=True)
            else:
                for b in range(SEQ_PER_CORE):
                    P.add(q, lambda e, b=b: e.dma_start(out=tile_[8 * b:8 * b + 8, :], in_=MODD[1 + b, sec * D:(sec + 1) * D].partition_broadcast(8)),
                          w=[key], dma=True)

        def norm_mod(P, xsrc_ap, xt, hbuf, ss, Gt, SHt, gkeys, tag, ps_banks, dst_fn, h_to=None):
            P.add('sp', lambda e: e.dma_start(out=xt[:], in_=xsrc_ap), w=[tag + 'x'], dma=True)
            P.add('act', lambda e: e.activation(out=hbuf[:], in_=xt[:], func=AF.Square, accum_out=ss[:, 0:1]), r=[tag + 'x'], w=[tag + 'h', tag + 'ss'])
            P.add('dve', lambda e: e.tensor_scalar(out=ss[:, 1:2], in0=ss[:, 0:1], scalar1=1.0 / D, scalar2=NORM_EPS, op0=ALU.mult, op1=ALU.add),
                  r=[tag + 'ss'], w=[tag + 'ss'])
            P.add('act', lambda e: e.sqrt(ss[:, 3:4], ss[:, 1:2]), r=[tag + 'ss'], w=[tag + 'ss'])
            P.add('dve', lambda e: e.reciprocal(out=ss[:, 2:3], in_=ss[:, 3:4]), r=[tag + 'ss'], w=[tag + 'ss'])
            P.add('dve', lambda e: e.scalar_tensor_tensor(out=hbuf[:], in0=xt[:], scalar=ss[:, 2:3], in1=Gt[:], op0=ALU.mult, op1=ALU.mult),
                  r=[tag + 'x', tag + 'ss', gkeys[0]], w=[tag + 'h'])
            P.add('dve', lambda e: e.tensor_tensor(out=hbuf[:], in0=hbuf[:], in1=SHt[:], op=ALU.add), r=[tag + 'h', gkeys[1]], w=[tag + 'h'])
            if h_to is not None:
                P.add('sp', lambda e: e.dma_start(out=h_to, in_=hbuf[:]), r=[tag + 'h'], w=[tag + 'hdram'], dma=True)
            for k4 in range(0, KD, 4):
                bank, bkey = ps_banks[(k4 // 4) % len(ps_banks)]
                nk = min(4, KD - k4)
                for kk in range(nk):
                    k = k4 + kk
                    P.add('pe', lambda e, k=k, kk=kk, bank=bank: e.transpose(bank[:, kk * 128:(kk + 1) * 128], hbuf[:, k * 128:(k + 1) * 128], ident),
                          r=[tag + 'h', 'tabs'], w=[bkey])
                dst, dkey = dst_fn(k4, nk)
                P.add('act', lambda e, bank=bank, dst=dst, nk=nk: e.activation(out=r32(dst), in_=bank[:, 0:nk * 128].rearrange("p (k t) -> p k t", k=nk), func=AF.Copy),
                      r=[bkey], w=[dkey])

        with ExitStack() as es:
            P = Prog(nc, st)
            NG1 = 2 if NPRE >= 2 else 1
            TG = NPRE // NG1
            hTp = sb(es, "hTp", [128, KD, TG * 128])
            kvp = sb(es, "kvp", [128, TG, 2 * RW])
            cup = sb(es, "cup", [128, 2 * CW])
            xts = [sb(es, "p1x%d" % i, [128, D]) for i in range(1)]
            hbs = [sb(es, "p1h%d" % i, [128, D]) for i in range(1)]
            sss = [sb(es, "p1s%d" % i, [128, 4]) for i in range(1)]
            G1 = sb(es, "p1G", [128, D])
            SH1 = sb(es, "p1SH", [128, D])
            wib = [sb(es, "p1w%d" % i, [128, KD, CB]) for i in range(2)]
            tmp = sb(es, "p1tmp", [128, 4, H, 64])
            banks = [(psb(es, "p1b%d" % i), ('p1b', i)) for i in range(8)]
            load_mod(P, G1, 'G1', 1, 0)
            load_mod(P, SH1, 'SH1', 0, 0)
            wir = w_in.rearrange("(k p) n -> p k n", p=128)
            o_c, _ = toff['prec']
            o_s, _ = toff['pres']
            o_kd, _ = toff['kdecpre']
            nj = 0
            for gi in range(NG1):
                tl = list(range(gi * TG, (gi + 1) * TG))
                for li, t in enumerate(tl):
                    norm_mod(P, xp[t * 128:(t + 1) * 128, :], xts[0], hbs[0], sss[0], G1, SH1, ('G1', 'SH1'), 'p1_',
                             banks[0:2], lambda k4, nk, li=li: (hTp[:, k4:k4 + nk, li * 128:(li + 1) * 128], ('hTp', li)))
                nkv = 2 * RW // CB
                jobs = [(RW // CB + j, list(range(TG)), j) for j in range(nkv)]
                cu0 = (4 * RW + CW) // CB
                if gi == NG1 - 1:
                    jobs += [(cu0 + j, [TG - 1], j) for j in range(2 * CW // CB)]
                for (cbi, lis, j) in jobs:
                    wb = wib[nj % 2]
                    wkey = ('p1w', nj % 2)
                    is_cu = cbi >= cu0
                    P.add('pool', lambda e, wb=wb, cbi=cbi: e.dma_start(out=r32(wb[:]), in_=wir[:, :, cbi * CB:(cbi + 1) * CB]), w=[wkey], dma=True)
                    for ti, li in enumerate(lis):
                        bank, bkey = banks[4 + (ti % 4)]
                        for k in range(KD):
                            P.add('pe', lambda e, k=k, li=li, wb=wb, bank=bank: e.matmul(bank[:, 0:CB], r32(hTp[:, k, li * 128:(li + 1) * 128]), r32(wb[:, k, :]),
                                                                                     start=(k == 0), stop=(k == KD - 1)),
                                  r=[('hTp', li), wkey], w=[bkey])
                        if is_cu:
                            dst, dk = cup[:, j * CB:(j + 1) * CB], 'cup'
                        else:
                            dst, dk = kvp[:, li, j * CB:(j + 1) * CB], ('kvp', li)
                        P.add('act' if ti % 2 == 0 else 'dve',
                              (lambda e, dst=dst, bank=bank: e.activation(out=dst, in_=bank[:, 0:CB], func=AF.Copy)) if ti % 2 == 0 else
                              (lambda e, dst=dst, bank=bank: e.tensor_copy(out=dst, in_=bank[:, 0:CB])),
                              r=[bkey], w=[dk])
                    nj += 1
                for li, t in enumerate(tl):
                    kk = kvp[:, li, 0:RW].rearrange("p (h two d) -> p h two d", h=H, two=2)
                    x1, x2 = kk[:, :, 0, :], kk[:, :, 1, :]
                    cs = bc_mid(tabs[:, o_c + t * 64:o_c + (t + 1) * 64], H)
                    sn = bc_mid(tabs[:, o_s + t * 64:o_s + (t + 1) * 64], H)
                    kv = ('kvp', li)
                    P.add('dve', lambda e, x1=x1, cs=cs: e.tensor_tensor(out=tmp[:, 0], in0=x1, in1=cs, op=ALU.mult), r=[kv, 'tabs'], w=['t0'])
                    P.add('pool', lambda e, x2=x2, sn=sn: e.tensor_tensor(out=tmp[:, 1], in0=x2, in1=sn, op=ALU.mult), r=[kv, 'tabs'], w=['t1'])
                    P.add('dve', lambda e, x1=x1, sn=sn: e.tensor_tensor(out=tmp[:, 2], in0=x1, in1=sn, op=ALU.mult), r=[kv, 'tabs'], w=['t2'])
                    P.add('pool', lambda e, x2=x2, cs=cs: e.tensor_tensor(out=tmp[:, 3], in0=x2, in1=cs, op=ALU.mult), r=[kv, 'tabs'], w=['t3'])
                    P.add('dve', lambda e, x1=x1: e.tensor_tensor(out=x1, in0=tmp[:, 0], in1=tmp[:, 1], op=ALU.subtract), r=['t0', 't1'], w=[kv])
                    P.add('dve', lambda e, x2=x2: e.tensor_tensor(out=x2, in0=tmp[:, 2], in1=tmp[:, 3], op=ALU.add), r=['t2', 't3'], w=[kv])
                    k3 = kvp[:, li, 0:RW].rearrange("p (h d) -> p h d", h=H)
                    kd = bc_last(tabs[:, o_kd + t * H:o_kd + (t + 1) * H], 128)
                    P.add('dve', lambda e, k3=k3, kd=kd: e.tensor_tensor(out=k3, in0=k3, in1=kd, op=ALU.mult), r=[kv, 'tabs'], w=[kv])
                sb_banks = banks[2:4]
                for h in range(H):
                    bank, bkey = sb_banks[(h // 4) % 2]
                    for li in range(TG):
                        P.add('pe', lambda e, h=h, li=li, bank=bank: e.matmul(bank[:, (h % 4) * 128:(h % 4 + 1) * 128], kvp[:, li, h * 128:(h + 1) * 128],
                                                                          kvp[:, li, RW + h * 128:RW + (h + 1) * 128], start=(li == 0), stop=(li == TG - 1)),
                              r=[('kvp', li)], w=[bkey])
                    if (h % 4 == 3) or h == H - 1:
                        g = h // 4
                        nh = h - 4 * g + 1
                        sv = S[:, 4 * g:4 * g + nh, :].rearrange("p h e -> p (h e)")
                        if gi == 0:
                            P.add('act', lambda e, bank=bank, sv=sv, nh=nh: e.activation(out=sv, in_=bank[:, 0:nh * 128], func=AF.Copy), r=[bkey], w=['S'])
                        else:
                            P.add('dve', lambda e, bank=bank, sv=sv, nh=nh: e.tensor_tensor(out=sv, in0=bank[:, 0:nh * 128], in1=sv, op=ALU.add), r=[bkey, 'S'], w=['S'])
            P.add('dve', lambda e: e.tensor_tensor(out=zprev[:], in0=cup[:, 0:CW], in1=cup[:, CW:2 * CW], op=ALU.mult), r=['cup'], w=['zprev'])
            if debug:
                P.add('sp', lambda e: e.dma_start(out=dbg['spre'], in_=S[:].rearrange("p h e -> p (h e)")), r=['S'], w=['dbgspre'], dma=True)
            P.emit()

        with ExitStack() as es:
            P = Prog(nc, st)
            hTm = sb(es, "hTm", [128, KD, NTOK])
            xts = [sb(es, "p2x%d" % i, [128, D]) for i in range(1)]
            hbs = [sb(es, "p2h%d" % i, [128, D]) for i in range(1)]
            sss = [sb(es, "p2s%d" % i, [128, 4]) for i in range(1)]
            Gt = [sb(es, "p2G%d" % i, [128, D]) for i in range(2)]
            SHt = [sb(es, "p2SH%d" % i, [128, D]) for i in range(2)]
            wib = [sb(es, "p2w%d" % i, [128, KD, CB]) for i in range(2)]
            stg = [sb(es, "p2st%d" % i, [128, CB]) for i in range(4)]
            banks = [(psb(es, "p2b%d" % i), ('p2b', i)) for i in range(8)]
            for ty in range(2):
                load_mod(P, Gt[ty], ('G', ty), 1, ty)
                load_mod(P, SHt[ty], ('SH', ty), 0, ty)
            for t in range(NT):
                i2 = 0
                ty = 0 if t < TP else 1
                norm_mod(P, xm[t * 128:(t + 1) * 128, :], xts[i2], hbs[i2], sss[i2], Gt[ty], SHt[ty], (('G', ty), ('SH', ty)), 'p2%d' % i2,
                         banks[0:4], lambda k4, nk, t=t: (hTm[:, k4:k4 + nk, t * 128:(t + 1) * 128], ('hTm', t)))
            wir = w_in.rearrange("(k p) n -> p k n", p=128)
            cnt = 0
            for cbi in range(IN // CB):
                wb = wib[cbi % 2]
                wkey = ('p2w', cbi % 2)
                P.add('pool', lambda e, wb=wb, cbi=cbi: e.dma_start(out=r32(wb[:]), in_=wir[:, :, cbi * CB:(cbi + 1) * CB]), w=[wkey], dma=True)
                for t in range(NT):
                    bank, bkey = banks[4 + (cnt % 4)]
                    sg_, skey = stg[cnt % 4], ('stg', cnt % 4)
                    for k in range(KD):
                        P.add('pe', lambda e, k=k, t=t, wb=wb, bank=bank: e.matmul(bank[:, 0:CB], r32(hTm[:, k, t * 128:(t + 1) * 128]), r32(wb[:, k, :]),
                                                                                 start=(k == 0), stop=(k == KD - 1)),
                              r=[('hTm', t), wkey], w=[bkey])
                    if cnt % 2 == 0:
                        P.add('act', lambda e, sg_=sg_, bank=bank: e.activation(out=sg_[:], in_=bank[:, 0:CB], func=AF.Copy), r=[bkey], w=[skey])
                    else:
                        P.add('dve', lambda e, sg_=sg_, bank=bank: e.tensor_copy(out=sg_[:], in_=bank[:, 0:CB]), r=[bkey], w=[skey])
                    P.add('pool', lambda e, sg_=sg_, t=t, cbi=cbi: e.dma_start(out=PROJ[t * 128:(t + 1) * 128, cbi * CB:(cbi + 1) * CB], in_=sg_[:]),
                          r=[skey], w=[('PROJ', t)], dma=True)
                    cnt += 1
            if debug:
                P.add('sp', lambda e: e.dma_start(out=dbg['proj'], in_=PROJ), r=[('PROJ', t) for t in range(NT)], w=['dbgproj'], dma=True)
            P.emit()

        with ExitStack() as es:
            P = Prog(nc, st)
            pj = sb(es, "pj", [128, IN])
            qkr = sb(es, "qkr", [128, 2 * H, 128])
            tmp = sb(es, "p3tmp", [128, 4, 2 * H, 64])
            qkT = sb(es, "qkT", [128, 2 * H, 128])
            smT = sb(es, "smT", [128, H, 128])
            osb = sb(es, "osb", [128, H, 128])
            osq = sb(es, "osq", [128, H, 128])
            stat = sb(es, "stat", [128, 6, H])
            gnb = sb(es, "gnb", [128, RW])
            cwb = sb(es, "cwb", [128, 3, CW])
            zp_s = sb(es, "zp_s", [128, CW])
            ctmp = sb(es, "ctmp", [128, CW])
            Zq = [sb(es, "Zq%d" % i, [128, SEQ_PER_CORE, 128]) for i in range(2)]
            kbZ = [sb(es, "kbZ%d" % i, [128, RW]) for i in range(2)]
            Sb = [sb(es, "Sb%d" % i, [128, H, 128]) for i in range(2)]
            Sold = [sb(es, "Sold%d" % i, [128, H, 128]) for i in range(2)]
            Ssh = [sb(es, "Ssh%d" % i, [128, SEQ_PER_CORE, 128]) for i in range(2)]
            banks = [(psb(es, "p3b%d" % i), ('p3b', i)) for i in range(8)]
            P.add('act', lambda e: e.dma_start(out=gnb[:], in_=ret_gn[0, :].partition_broadcast(128)), w=['gnb'], dma=True)
            P.add('act', lambda e: e.dma_start(out=cwb[:].rearrange("p a c -> p (a c)"), in_=conv_w[0, :].partition_broadcast(128)), w=['cwb'], dma=True)
            P.add('pool', lambda e: e.memset(zp_s[:], 0.0), w=['zp_s'])
            P.add('act', lambda e: e.dma_start(out=zp_s[0:32, :], in_=sconv), r=['zp_s'], w=['zp_s'], dma=True)
            for i in range(2):
                P.add('pool', lambda e, i=i: e.memset(Zq[i][:], 0.0), w=[('Zq', i)])
            o_c, _ = toff['ropec']
            o_s, _ = toff['ropes']
            o_dec, _ = toff['dec']
            o_gL, _ = toff['gL']
            o_m, _ = toff['maskT']
            o_cm, _ = toff['convm']
            o_sm, _ = toff['seqmask']
            QO, KO, VO, GO, BO, CO, UO = 0, RW, 2 * RW, 3 * RW, 4 * RW, 4 * RW + CW, 4 * RW + 2 * CW
            nHG = (H + 3) // 4
            for t in range(NT):
                ty = 0 if t < TP else 1
                cty = (0 if t == 0 else 1) if ty == 0 else 2
                P.add('sp', lambda e, t=t: e.dma_start(out=pj[:], in_=PROJ[t * 128:(t + 1) * 128, :]), w=['pj_q', 'pj_v', 'pj_g', 'pj_B', 'pj_C', 'pj_u'], dma=True)
                qk = pj[:, 0:2 * RW].rearrange("p (h two d) -> p h two d", h=2 * H, two=2)
                x1, x2 = qk[:, :, 0, :], qk[:, :, 1, :]
                ov = qkr[:].rearrange("p h (two d) -> p h two d", two=2)
                cs = bc_mid(tabs[:, o_c + t * 64:o_c + (t + 1) * 64], 2 * H)
                sn = bc_mid(tabs[:, o_s + t * 64:o_s + (t + 1) * 64], 2 * H)
                P.add('dve', lambda e, x1=x1, cs=cs: e.tensor_tensor(out=tmp[:, 0], in0=x1, in1=cs, op=ALU.mult), r=['pj_q', 'tabs'], w=['t0'])
                P.add('pool', lambda e, x2=x2, sn=sn: e.tensor_tensor(out=tmp[:, 1], in0=x2, in1=sn, op=ALU.mult), r=['pj_q', 'tabs'], w=['t1'])
                P.add('dve', lambda e, x1=x1, sn=sn: e.tensor_tensor(out=tmp[:, 2], in0=x1, in1=sn, op=ALU.mult), r=['pj_q', 'tabs'], w=['t2'])
                P.add('pool', lambda e, x2=x2, cs=cs: e.tensor_tensor(out=tmp[:, 3], in0=x2, in1=cs, op=ALU.mult), r=['pj_q', 'tabs'], w=['t3'])
                P.add('dve', lambda e, ov=ov: e.tensor_tensor(out=ov[:, :, 0, :], in0=tmp[:, 0], in1=tmp[:, 1], op=ALU.subtract), r=['t0', 't1'], w=['qkr'])
                P.add('dve', lambda e, ov=ov: e.tensor_tensor(out=ov[:, :, 1, :], in0=tmp[:, 2], in1=tmp[:, 3], op=ALU.add), r=['t2', 't3', 'qkr'], w=['qkr'])
                dc = bc_last(tabs[:, o_dec + ty * 2 * H:o_dec + (ty + 1) * 2 * H], 128)
                P.add('dve', lambda e, dc=dc: e.tensor_tensor(out=qkr[:], in0=qkr[:], in1=dc, op=ALU.mult), r=['qkr', 'tabs'], w=['qkr'])
                for g in range((2 * H + 3) // 4):
                    bank, bkey = banks[g % 4]
                    nh = min(4, 2 * H - 4 * g)
                    for hh in range(nh):
                        P.add('pe', lambda e, g=g, hh=hh, bank=bank: e.transpose(bank[:, hh * 128:(hh + 1) * 128], qkr[:, 4 * g + hh, :], ident),
                              r=['qkr', 'tabs'], w=[bkey])
                    P.add('act', lambda e, g=g, nh=nh, bank=bank: e.activation(out=qkT[:, 4 * g:4 * g + nh, :].rearrange("p h t -> p (h t)"), in_=bank[:, 0:nh * 128], func=AF.Copy),
                          r=[bkey], w=['qkT'])
                mk = bc_mid(tabs[:, o_m + ty * 128:o_m + (ty + 1) * 128], 4)
                for g in range(nHG):
                    bank, bkey = banks[4 + g % 2]
                    nh = min(4, H - 4 * g)
                    for hh in range(nh):
                        h = 4 * g + hh
                        P.add('pe', lambda e, h=h, hh=hh, bank=bank: e.matmul(bank[:, hh * 128:(hh + 1) * 128], qkT[:, H + h, :], qkT[:, h, :], start=True, stop=True),
                              r=['qkT'], w=[bkey])
                    P.add('dve', lambda e, g=g, nh=nh, bank=bank, mk=mk: e.tensor_tensor(out=smT[:, 4 * g:4 * g + nh, :], in0=bank[:, 0:nh * 128].rearrange("p (h t) -> p h t", h=nh),
                                                                                    in1=mk[:, 0:nh, :], op=ALU.mult),
                          r=[bkey, 'tabs'], w=['smT'])
                obanks = [banks[6], banks[7]]
                for h in range(H):
                    bank, bkey = obanks[(h // 4) % 2]
                    oreg = bank[:, (h % 4) * 128:(h % 4 + 1) * 128]
                    vh = pj[:, VO + h * 128:VO + (h + 1) * 128]
                    if ty == 0:
                        P.add('pe', lambda e, h=h, oreg=oreg, vh=vh: e.matmul(oreg, smT[:, h, :], vh, start=True, stop=False), r=['smT', 'pj_v'], w=[bkey])
                        P.add('pe', lambda e, h=h, oreg=oreg: e.matmul(oreg, qkT[:, h, :], S[:, h, :], start=False, stop=True), r=['qkT', 'S'], w=[bkey])
                    else:
                        zq, zk = Zq[h % 2], ('Zq', h % 2)
                        ssh, sshk = Ssh[h % 2], ('Ssh', h % 2)
                        P.add('act', lambda e, h=h, ssh=ssh: e.dma_start(out=ssh[:], in_=sret[:, h, :, :].rearrange("b d e -> d b e")), w=[sshk], dma=True)
                        P.add('dve', lambda e, h=h, zq=zq: e.tensor_copy(
                            out=bass.AP(zq[:].tensor, zq[:].offset, [list(zq[:].ap[0]), [136, SEQ_PER_CORE], [1, 8]]),
                            in_=qkT[:, h, :].rearrange("p (b i) -> p b i", i=8)), r=['qkT', zk], w=[zk])
                        P.add('pe', lambda e, h=h, oreg=oreg, vh=vh: e.matmul(oreg, smT[:, h, :], vh, start=True, stop=False), r=['smT', 'pj_v'], w=[bkey])
                        for b in range(SEQ_PER_CORE):
                            P.add('pe', lambda e, h=h, b=b, oreg=oreg, zq=zq, ssh=ssh: e.matmul(oreg, zq[:, b, :], ssh[:, b, :], start=False, stop=(b == SEQ_PER_CORE - 1)),
                                  r=[zk, sshk], w=[bkey])
                    if (h % 4 == 3) or h == H - 1:
                        g = h // 4
                        nh = h - 4 * g + 1
                        P.add('act', lambda e, g=g, nh=nh, bank=bank: e.activation(out=osb[:, 4 * g:4 * g + nh, :].rearrange("p h e -> p (h e)"), in_=bank[:, 0:nh * 128], func=AF.Copy),
                              r=[bkey], w=['osb'])
                if ty == 0:
                    for g in range(nHG):
                        bank, bkey = banks[4 + g % 2]
                        nh = min(4, H - 4 * g)
                        for hh in range(nh):
                            h = 4 * g + hh
                            P.add('pe', lambda e, h=h, hh=hh, bank=bank: e.matmul(bank[:, hh * 128:(hh + 1) * 128], qkr[:, H + h, :], pj[:, VO + h * 128:VO + (h + 1) * 128], start=True, stop=True),
                                  r=['qkr', 'pj_v'], w=[bkey])
                        P.add('dve', lambda e, g=g, nh=nh, bank=bank: e.tensor_tensor(out=S[:, 4 * g:4 * g + nh, :], in0=bank[:, 0:nh * 128].rearrange("p (h t) -> p h t", h=nh),
                                                                                in1=S[:, 4 * g:4 * g + nh, :], op=ALU.add), r=[bkey, 'S'], w=['S'])
                    gl = bc_last(tabs[:, o_gL:o_gL + H], 128)
                    P.add('dve', lambda e, gl=gl: e.tensor_tensor(out=S[:], in0=S[:], in1=gl, op=ALU.mult), r=['S', 'tabs'], w=['S'])
                    if t == TP - 1:
                        P.add('sp', lambda e: e.dma_start(out=ret_p.rearrange("h d e -> d h e"), in_=S[:]), r=['S'], w=['ret_p'], dma=True)
                else:
                    gl = bc_last(tabs[:, o_gL + H:o_gL + 2 * H], 128)
                    for b in range(SEQ_PER_CORE):
                        kz, kzk = kbZ[b % 2], ('kbZ', b % 2)
                        sbb, sbk = Sb[b % 2], ('Sb', b % 2)
                        so, sok = Sold[b % 2], ('Sold', b % 2)
                        P.add('act', lambda e, b=b, so=so: e.dma_start(out=so[:], in_=sret[b].rearrange("h d e -> d h e")), w=[sok], dma=True)
                        P.add('pool', lambda e, b=b, kz=kz: e.tensor_scalar(out=kz[:], in0=qkr[:, H:2 * H, :].rearrange("p h d -> p (h d)"),
                                                                            scalar1=tabs[:, o_sm + b:o_sm + b + 1], scalar2=None, op0=ALU.mult),
                              r=['qkr', 'tabs'], w=[kzk])
                        for g in range(nHG):
                            bank, bkey = banks[4 + g % 2]
                            nh = min(4, H - 4 * g)
                            for hh in range(nh):
                                h = 4 * g + hh
                                P.add('pe', lambda e, h=h, hh=hh, bank=bank, kz=kz: e.matmul(bank[:, hh * 128:(hh + 1) * 128], kz[:, h * 128:(h + 1) * 128],
                                                                                     pj[:, VO + h * 128:VO + (h + 1) * 128], start=True, stop=True),
                                      r=[kzk, 'pj_v'], w=[bkey])
                            P.add('dve', lambda e, g=g, nh=nh, bank=bank, b=b, sbb=sbb, so=so: e.tensor_tensor(out=sbb[:, 4 * g:4 * g + nh, :], in0=bank[:, 0:nh * 128].rearrange("p (h t) -> p h t", h=nh),
                                                                                              in1=so[:, 4 * g:4 * g + nh, :], op=ALU.add),
                                  r=[bkey, sok], w=[sbk])
                        P.add('dve', lambda e, sbb=sbb, gl=gl: e.tensor_tensor(out=sbb[:], in0=sbb[:], in1=gl, op=ALU.mult), r=[sbk, 'tabs'], w=[sbk])
                        P.add('sp', lambda e, b=b, sbb=sbb: e.dma_start(out=ret_s[b].rearrange("h d e -> d h e"), in_=sbb[:]), r=[sbk], w=[('ret_s', b)], dma=True)
                P.add('dve', lambda e: e.tensor_reduce(out=stat[:, 0, :], in_=osb[:], axis=AX.X, op=ALU.add), r=['osb'], w=['st0'])
                P.add('pool', lambda e: e.tensor_tensor(out=osq[:], in0=osb[:], in1=osb[:], op=ALU.mult), r=['osb'], w=['osq'])
                P.add('dve', lambda e: e.tensor_reduce(out=stat[:, 1, :], in_=osq[:], axis=AX.X, op=ALU.add), r=['osq'], w=['st1'])
                P.add('dve', lambda e: e.tensor_scalar(out=stat[:, 2, :], in0=stat[:, 0, :], scalar1=1.0 / 128, scalar2=None, op0=ALU.mult), r=['st0'], w=['st2'])
                P.add('dve', lambda e: e.tensor_tensor(out=stat[:, 3, :], in0=stat[:, 2, :], in1=stat[:, 2, :], op=ALU.mult), r=['st2'], w=['st3'])
                P.add('dve', lambda e: e.scalar_tensor_tensor(out=stat[:, 4, :], in0=stat[:, 1, :], scalar=1.0 / 128, in1=stat[:, 3, :], op0=ALU.mult, op1=ALU.subtract),
                      r=['st1', 'st3'], w=['st4'])
                P.add('dve', lambda e: e.tensor_scalar(out=stat[:, 5, :], in0=stat[:, 4, :], scalar1=NORM_EPS, scalar2=None, op0=ALU.add), r=['st4'], w=['st5'])
                P.add('act', lambda e: e.sqrt(stat[:, 5, :], stat[:, 5, :]), r=['st5'], w=['st5'])
                P.add('dve', lambda e: e.reciprocal(out=stat[:, 5, :], in_=stat[:, 5, :]), r=['st5'], w=['st5'])
                P.add('dve', lambda e: e.tensor_tensor(out=osb[:], in0=osb[:], in1=bc_last(stat[:, 2, :], 128), op=ALU.subtract), r=['osb', 'st2', 'osq'], w=['osb'])
                P.add('dve', lambda e: e.tensor_tensor(out=osb[:], in0=osb[:], in1=bc_last(stat[:, 5, :], 128), op=ALU.mult), r=['osb', 'st5'], w=['osb'])
                P.add('pool', lambda e: e.tensor_tensor(out=osb[:].rearrange("p h e -> p (h e)"), in0=osb[:].rearrange("p h e -> p (h e)"), in1=gnb[:], op=ALU.mult), r=['osb', 'gnb'], w=['osb'])
                P.add('act', lambda e: e.activation(out=pj[:, GO:GO + RW], in_=pj[:, GO:GO + RW], func=AF.Silu), r=['pj_g'], w=['pj_g'])
                P.add('dve', lambda e: e.tensor_tensor(out=pj[:, GO:GO + RW], in0=pj[:, GO:GO + RW], in1=osb[:].rearrange("p h e -> p (h e)"), op=ALU.mult), r=['pj_g', 'osb'], w=['pj_g'])
                zz = pj[:, CO:CO + CW]
                P.add('pool', lambda e, zz=zz: e.tensor_tensor(out=zz, in0=zz, in1=pj[:, UO:UO + CW], op=ALU.mult), r=['pj_C', 'pj_u'], w=['pj_C'])
                zpv = zprev if ty == 0 else zp_s
                zpk = 'zprev' if ty == 0 else 'zp_s'
                cmv = tabs[:, o_cm + cty * 512:o_cm + (cty + 1) * 512].rearrange("p (a t) -> p a t", a=4)
                shb = [banks[0], banks[1], banks[2], banks[3]]
                nq = (CW + 511) // 512
                for sh in range(2):
                    for q in range(nq):
                        bank, bkey = shb[sh * 2 + q % 2]
                        wq = min(512, CW - q * 512)
                        P.add('pe', lambda e, sh=sh, q=q, wq=wq, bank=bank, cmv=cmv, zz=zz: e.matmul(bank[:, 0:wq], cmv[:, sh, :], zz[:, q * 512:q * 512 + wq], start=True, stop=False),
                              r=['tabs', 'pj_C'], w=[bkey])
                        P.add('pe', lambda e, sh=sh, q=q, wq=wq, bank=bank, cmv=cmv, zpv=zpv: e.matmul(bank[:, 0:wq], cmv[:, 2 + sh, :], zpv[:, q * 512:q * 512 + wq], start=False, stop=True),
                              r=['tabs', zpk], w=[bkey])
                        cs_ = slice(q * 512, q * 512 + wq)
                        if sh == 0:
                            P.add('dve', lambda e, bank=bank, wq=wq, cs_=cs_: e.tensor_tensor(out=ctmp[:, cs_], in0=bank[:, 0:wq], in1=cwb[:, 1, cs_], op=ALU.mult),
                                  r=[bkey, 'cwb'], w=[('ctmp', q)])
                        else:
                            P.add('dve', lambda e, bank=bank, wq=wq, cs_=cs_: e.tensor_tensor(out=pj[:, UO + cs_.start:UO + cs_.stop], in0=bank[:, 0:wq], in1=cwb[:, 0, cs_], op=ALU.mult),
                                  r=[bkey, 'cwb', 'pj_C'], w=[('pj_u2', q)])
                P.add('pool', lambda e: e.tensor_tensor(out=ctmp[:], in0=ctmp[:], in1=pj[:, UO:UO + CW], op=ALU.add), r=[('ctmp', q) for q in range(nq)] + [('pj_u2', q) for q in range(nq)], w=['ctmpf'])
                P.add('dve', lambda e, zz=zz: e.tensor_tensor(out=pj[:, UO:UO + CW], in0=zz, in1=cwb[:, 2, :], op=ALU.mult), r=['pj_C', 'cwb', 'ctmpf'], w=['pj_u'])
                P.add('dve', lambda e: e.tensor_tensor(out=ctmp[:], in0=ctmp[:], in1=pj[:, UO:UO + CW], op=ALU.add), r=['ctmpf', 'pj_u'], w=['ctmpf'])
                P.add('dve', lambda e: e.tensor_tensor(out=pj[:, BO:BO + CW], in0=pj[:, BO:BO + CW], in1=ctmp[:], op=ALU.mult), r=['pj_B', 'ctmpf'], w=['pj_B'])
                if ty == 0:
                    P.add('act', lambda e, zz=zz: e.activation(out=zprev[:], in_=zz, func=AF.Copy), r=['pj_C', 'zprev'], w=['zprev'])
                    if t == TP - 1:
                        P.add('sp', lambda e: e.dma_start(out=conv_p, in_=zprev[126:128, :]), r=['zprev'], w=['conv_p'], dma=True)
                else:
                    P.add('sp', lambda e, zz=zz: e.dma_start(out=conv_s, in_=zz), r=['pj_C'], w=['conv_s'], dma=True)
                P.add('sp', lambda e, t=t: e.dma_start(out=MIX[t * 128:(t + 1) * 128, :], in_=pj[:, GO:GO + RW + CW]), r=['pj_g', 'pj_B'], w=[('MIX', t)], dma=True)
            P.emit()


        with ExitStack() as es:
            P = Prog(nc, st)
            GS = 3
            mixT = sb(es, "mixT", [128, KD, GS * 128])
            mxs = [sb(es, "mxs%d" % i, [128, D]) for i in range(2)]
            x1g = [sb(es, "x1g%d" % i, [128, D]) for i in range(GS)]
            wob = [sb(es, "wob%d" % i, [128, KD, CB]) for i in range(2)]
            g1t = sb(es, "g1t", [128, D])
            G2t = sb(es, "G2t", [128, D])
            SH2t = sb(es, "SH2t", [128, D])
            h2b = [sb(es, "h2b%d" % i, [128, D]) for i in range(2)]
            h2T = [sb(es, "h2T%d" % i, [128, KD, 128]) for i in range(2)]
            ss4 = [sb(es, "ss4%d" % i, [128, 4]) for i in range(2)]
            tmp4 = [sb(es, "tmp4%d" % i, [128, CB]) for i in range(2)]
            wr = sb(es, "wr", [128, KD, E])
            brb = sb(es, "brb", [128, E])
            banks = [(psb(es, "p4b%d" % i), ('p4b', i)) for i in range(8)]
            P.add('act', lambda e: e.dma_start(out=wr[:], in_=w_router.rearrange("(k p) n -> p k n", p=128)), w=['wr'], dma=True)
            P.add('act', lambda e: e.dma_start(out=brb[:], in_=b_router[0, :].partition_broadcast(128)), w=['brb'], dma=True)
            wor = w_o.rearrange("(k p) n -> p k n", p=128)
            groups = [list(range(g0, min(g0 + GS, NT))) for g0 in range(0, NT, GS)]
            cur_ty = -1
            wcnt = 0
            ecnt = 0
            for grp in groups:
                sub = [[t for t in grp if t < TP], [t for t in grp if t >= TP]]
                for ty, tl in enumerate(sub):
                    if not tl:
                        continue
                    if ty != cur_ty:
                        load_mod(P, g1t, 'g1t', 2, ty)
                        load_mod(P, G2t, 'G2t', 4, ty)
                        load_mod(P, SH2t, 'SH2t', 3, ty)
                        cur_ty = ty
                    for li, t in enumerate(tl):
                        ms, mk_ = mxs[li % 2], ('mxs', li % 2)
                        P.add('sp', lambda e, t=t, ms=ms: e.dma_start(out=ms[:], in_=MIX[t * 128:(t + 1) * 128, :]), w=[mk_], dma=True)
                        P.add('sp', lambda e, t=t, li=li: e.dma_start(out=x1g[li][:], in_=xm[t * 128:(t + 1) * 128, :]), w=[('x1g', li)], dma=True)
                        for k4 in range(0, KD, 4):
                            bank, bkey = banks[(k4 // 4) % 4]
                            nk = min(4, KD - k4)
                            for kk in range(nk):
                                P.add('pe', lambda e, k=k4 + kk, kk=kk, bank=bank, ms=ms: e.transpose(bank[:, kk * 128:(kk + 1) * 128], ms[:, k * 128:(k + 1) * 128], ident),
                                      r=[mk_, 'tabs'], w=[bkey])
                            P.add('act', lambda e, bank=bank, k4=k4, nk=nk, li=li: e.activation(out=r32(mixT[:, k4:k4 + nk, li * 128:(li + 1) * 128]),
                                                                                        in_=bank[:, 0:nk * 128].rearrange("p (k t) -> p k t", k=nk), func=AF.Copy),
                                  r=[bkey], w=[('mixT', li)])
                    for cbi in range(D // CB):
                        wb, wkey = wob[wcnt % 2], ('wob', wcnt % 2)
                        wcnt += 1
                        P.add('pool', lambda e, wb=wb, cbi=cbi: e.dma_start(out=r32(wb[:]), in_=wor[:, :, cbi * CB:(cbi + 1) * CB]), w=[wkey], dma=True)
                        for li, t in enumerate(tl):
                            bank, bkey = banks[4 + ecnt % 2]
                            tm, tmk = tmp4[ecnt % 2], ('tmp4', ecnt % 2)
                            ecnt += 1
                            for k in range(KD):
                                P.add('pe', lambda e, k=k, li=li, wb=wb, bank=bank: e.matmul(bank[:, 0:CB], r32(mixT[:, k, li * 128:(li + 1) * 128]), r32(wb[:, k, :]),
                                                                                          start=(k == 0), stop=(k == KD - 1)),
                                      r=[('mixT', li), wkey], w=[bkey])
                            cs_ = slice(cbi * CB, (cbi + 1) * CB)
                            P.add('dve', lambda e, bank=bank, tm=tm, cs_=cs_: e.tensor_tensor(out=tm[:], in0=bank[:, 0:CB], in1=g1t[:, cs_], op=ALU.mult), r=[bkey, 'g1t'], w=[tmk])
                            P.add('pool', lambda e, tm=tm, li=li, cs_=cs_: e.tensor_tensor(out=x1g[li][:, cs_], in0=x1g[li][:, cs_], in1=tm[:], op=ALU.add), r=[tmk, ('x1g', li)], w=[('x1g', li)])
                    for li, t in enumerate(tl):
                        i2 = t % 2
                        xk = ('x1g', li)
                        P.add('sp', lambda e, t=t, li=li: e.dma_start(out=X1[t * 128:(t + 1) * 128, :], in_=x1g[li][:]), r=[xk], w=[('X1', t)], dma=True)
                        hb, hk = h2b[i2], ('h2b', i2)
                        ss, sk = ss4[i2], ('ss4', i2)
                        P.add('act', lambda e, li=li, hb=hb, ss=ss: e.activation(out=hb[:], in_=x1g[li][:], func=AF.Square, accum_out=ss[:, 0:1]), r=[xk], w=[hk, sk])
                        P.add('dve', lambda e, ss=ss: e.tensor_scalar(out=ss[:, 1:2], in0=ss[:, 0:1], scalar1=1.0 / D, scalar2=NORM_EPS, op0=ALU.mult, op1=ALU.add), r=[sk], w=[sk])
                        P.add('act', lambda e, ss=ss: e.sqrt(ss[:, 3:4], ss[:, 1:2]), r=[sk], w=[sk])
                        P.add('dve', lambda e, ss=ss: e.reciprocal(out=ss[:, 2:3], in_=ss[:, 3:4]), r=[sk], w=[sk])
                        P.add('dve', lambda e, li=li, hb=hb, ss=ss: e.scalar_tensor_tensor(out=hb[:], in0=x1g[li][:], scalar=ss[:, 2:3], in1=G2t[:], op0=ALU.mult, op1=ALU.mult),
                              r=[xk, sk, 'G2t'], w=[hk])
                        P.add('dve', lambda e, hb=hb: e.tensor_tensor(out=hb[:], in0=hb[:], in1=SH2t[:], op=ALU.add), r=[hk, 'SH2t'], w=[hk])
                        P.add('sp', lambda e, t=t, hb=hb: e.dma_start(out=H2[t * 128:(t + 1) * 128, :], in_=hb[:]), r=[hk], w=[('H2', t)], dma=True)
                        hT, hTk = h2T[i2], ('h2T', i2)
                        for k4 in range(0, KD, 4):
                            bank, bkey = banks[(k4 // 4) % 4]
                            nk = min(4, KD - k4)
                            for kk in range(nk):
                                P.add('pe', lambda e, k=k4 + kk, kk=kk, bank=bank, hb=hb: e.transpose(bank[:, kk * 128:(kk + 1) * 128], hb[:, k * 128:(k + 1) * 128], ident),
                                      r=[hk, 'tabs'], w=[bkey])
                            P.add('act', lambda e, bank=bank, k4=k4, nk=nk, hT=hT: e.activation(out=hT[:, k4:k4 + nk, :], in_=bank[:, 0:nk * 128].rearrange("p (k t) -> p k t", k=nk), func=AF.Copy),
                                  r=[bkey], w=[hTk])
                        bank, bkey = banks[6 + i2]
                        for k in range(KD):
                            P.add('pe', lambda e, k=k, hT=hT, bank=bank: e.matmul(bank[:, 0:E], hT[:, k, :], wr[:, k, :], start=(k == 0), stop=(k == KD - 1)), r=[hTk, 'wr'], w=[bkey])
                        P.add('dve', lambda e, t=t, bank=bank: e.tensor_tensor(out=logits[:, t, :], in0=bank[:, 0:E], in1=brb[:], op=ALU.add), r=[bkey, 'brb'], w=['logits'])
            if debug:
                P.add('sp', lambda e: e.dma_start(out=dbg['mix'], in_=MIX), w=['dbgmix'], dma=True)
                P.add('sp', lambda e: e.dma_start(out=dbg['x1'], in_=X1), r=[('X1', t) for t in range(NT)], w=['dbgx1'], dma=True)
                P.add('sp', lambda e: e.dma_start(out=dbg['h2'], in_=H2), r=[('H2', t) for t in range(NT)], w=['dbgh2'], dma=True)
                P.add('sp', lambda e: e.dma_start(out=dbg['logits'], in_=logits[:].rearrange("p t e -> p (t e)")), r=['logits'], w=['dbglog'], dma=True)
            P.emit()

        with ExitStack() as es:
            P = Prog(nc, st)
            NE = NT * E
            rem = sb(es, "rem", [128, NT, E])
            oh = [sb(es, "oh%d" % k, [128, NT, E]) for k in range(TOPK)]
            mask = sb(es, "mask", [128, NT, E])
            ex = sb(es, "ex", [128, NT, E])
            gates = sb(es, "gates", [128, NT, E])
            rank = sb(es, "rank", [128, NT, E])
            t3 = sb(es, "t3", [128, NT, E])
            vk = sb(es, "vk", [128, 8, NT])
            t4 = sb(es, "t4", [128, NT])
            gf = sb(es, "gf", [128, NT, TOPK])
            Pm = [sb(es, "Pm%d" % i, [128, NT, CAP]) for i in range(2)]
            idxf = sb(es, "idxf", [128, E * NS])
            banks = [(psb(es, "p5b%d" % i), ('p5b', i)) for i in range(8)]
            o_U, _ = toff['U']
            o_1, _ = toff['ONES']
            o_ic, _ = toff['iotac']
            o_ie, _ = toff['iotaE']
            o_tk, _ = toff['tokid']
            Ut, ONt = tabs[:, o_U:o_U + 128], tabs[:, o_1:o_1 + 128]
            P.add('dve', lambda e: e.tensor_copy(out=rem[:], in_=logits[:]), r=['logits'], w=['rem'])
            P.add('dve', lambda e: e.tensor_reduce(out=vk[:, 4, :], in_=logits[:], axis=AX.X, op=ALU.max), r=['logits'], w=['m1'])
            for k in range(TOPK):
                P.add('dve', lambda e, k=k: e.tensor_reduce(out=vk[:, k, :], in_=rem[:], axis=AX.X, op=ALU.max), r=['rem'], w=[('vk', k)])
                P.add('dve', lambda e, k=k: e.tensor_tensor(out=oh[k][:], in0=rem[:], in1=bc_last(vk[:, k, :], E), op=ALU.is_equal), r=['rem', ('vk', k)], w=[('oh', k)])
                P.add('dve', lambda e, k=k: e.scalar_tensor_tensor(out=rem[:], in0=oh[k][:], scalar=-1e30, in1=rem[:], op0=ALU.mult, op1=ALU.add), r=[('oh', k), 'rem'], w=['rem'])
            P.add('dve', lambda e: e.tensor_tensor(out=mask[:], in0=oh[0][:], in1=oh[1][:], op=ALU.add), r=[('oh', 0), ('oh', 1)], w=['mask'])
            P.add('dve', lambda e: e.tensor_tensor(out=mask[:], in0=mask[:], in1=oh[2][:], op=ALU.add), r=[('oh', 2), 'mask'], w=['mask'])
            P.add('dve', lambda e: e.tensor_tensor(out=mask[:], in0=mask[:], in1=oh[3][:], op=ALU.add), r=[('oh', 3), 'mask'], w=['mask'])
            P.add('dve', lambda e: e.tensor_tensor(out=ex[:], in0=logits[:], in1=bc_last(vk[:, 4, :], E), op=ALU.subtract), r=['logits', 'm1'], w=['ex'])
            P.add('act', lambda e: e.activation(out=ex[:], in_=ex[:], func=AF.Exp), r=['ex'], w=['ex'])
            P.add('dve', lambda e: e.tensor_tensor(out=ex[:], in0=ex[:], in1=mask[:], op=ALU.mult), r=['ex', 'mask'], w=['ex'])
            P.add('dve', lambda e: e.tensor_reduce(out=vk[:, 5, :], in_=ex[:], axis=AX.X, op=ALU.add), r=['ex'], w=['den'])
            P.add('dve', lambda e: e.reciprocal(out=vk[:, 6, :], in_=vk[:, 5, :]), r=['den'], w=['rden'])
            P.add('dve', lambda e: e.tensor_tensor(out=gates[:], in0=ex[:], in1=bc_last(vk[:, 6, :], E), op=ALU.mult), r=['ex', 'rden'], w=['gates'])
            for t in range(NT):
                bank, bkey = banks[t % 2]
                P.add('pe', lambda e, t=t, bank=bank: e.matmul(bank[:, 0:E], Ut, mask[:, t, :], start=True, stop=(t == 0)), r=['mask', 'tabs'], w=[bkey])
                for t2 in range(t):
                    P.add('pe', lambda e, t2=t2, t=t, bank=bank: e.matmul(bank[:, 0:E], ONt, mask[:, t2, :], start=False, stop=(t2 == t - 1)), r=['mask', 'tabs'], w=[bkey])
                P.add('act', lambda e, t=t, bank=bank: e.activation(out=rank[:, t, :], in_=bank[:, 0:E], func=AF.Copy), r=[bkey], w=['rank'])
            ioE = bc_mid(tabs[:, o_ie:o_ie + E], NT)
            for k in range(TOPK):
                P.add('dve', lambda e, k=k: e.tensor_tensor(out=t3[:], in0=oh[k][:], in1=gates[:], op=ALU.mult), r=[('oh', k), 'gates', 't3'], w=['t3'])
                P.add('dve', lambda e, k=k: e.tensor_reduce(out=wk[:, :, k], in_=t3[:], axis=AX.X, op=ALU.add), r=['t3'], w=['wk'])
                P.add('dve', lambda e, k=k: e.tensor_tensor(out=t3[:], in0=oh[k][:], in1=ioE, op=ALU.mult), r=[('oh', k), 'tabs', 'wk'], w=['t3'])
                P.add('dve', lambda e, k=k: e.tensor_reduce(out=vk[:, 7, :], in_=t3[:], axis=AX.X, op=ALU.add), r=['t3'], w=['ek'])
                P.add('dve', lambda e, k=k: e.tensor_tensor(out=t3[:], in0=oh[k][:], in1=rank[:], op=ALU.mult), r=[('oh', k), 'rank', 'ek'], w=['t3'])
                P.add('dve', lambda e, k=k: e.tensor_reduce(out=gf[:, :, k], in_=t3[:], axis=AX.X, op=ALU.add), r=['t3'], w=['gf'])
                P.add('dve', lambda e, k=k: e.tensor_scalar(out=t4[:], in0=gf[:, :, k], scalar1=float(CAP) - 0.5, scalar2=None, op0=ALU.is_lt), r=['gf'], w=['t4'])
                P.add('dve', lambda e, k=k: e.tensor_tensor(out=wk[:, :, k], in0=wk[:, :, k], in1=t4[:], op=ALU.mult), r=['t4', 'wk'], w=['wk'])
                P.add('dve', lambda e, k=k: e.tensor_scalar(out=gf[:, :, k], in0=gf[:, :, k], scalar1=float(CAP - 1), scalar2=None, op0=ALU.min), r=['gf', 't4'], w=['gf'])
                P.add('dve', lambda e, k=k: e.scalar_tensor_tensor(out=gf[:, :, k], in0=vk[:, 7, :], scalar=float(CAP), in1=gf[:, :, k], op0=ALU.mult, op1=ALU.add), r=['ek', 'gf'], w=['gf'])
            P.add('dve', lambda e: e.tensor_copy(out=gidx_i[:], in_=gf[:].rearrange("p t k -> p (t k)")), r=['gf'], w=['gidx_i'])
            ioc = tabs[:, o_ic:o_ic + CAP]
            for ee in range(E):
                pm, pmk = Pm[ee % 2], ('Pm', ee % 2)
                for t in range(NT):
                    P.add('dve', lambda e, ee=ee, t=t, pm=pm: e.tensor_scalar(out=pm[:, t, :], in0=ioc, scalar1=rank[:, t, ee:ee + 1], scalar2=mask[:, t, ee:ee + 1],
                                                                       op0=ALU.is_equal, op1=ALU.mult), r=['rank', 'mask', 'tabs'], w=[(pmk, t)])
                for s_ in range(NS):
                    col = ee * NS + s_
                    bank, bkey = banks[2 + (col // 4) % 4]
                    for t in range(NT):
                        P.add('pe', lambda e, t=t, s_=s_, pm=pm, bank=bank, col=col: e.matmul(bank[:, (col % 4) * 2:(col % 4) * 2 + 2], pm[:, t, s_ * 128:(s_ + 1) * 128],
                                                                                      tabs[:, o_tk + t:o_tk + t + 1].to_broadcast([128, 2]), start=(t == 0), stop=(t == NT - 1)),
                              r=[(pmk, t), 'tabs'], w=[bkey])
                    P.add('act', lambda e, col=col, bank=bank: e.activation(out=idxf[:, col:col + 1], in_=bank[:, (col % 4) * 2:(col % 4) * 2 + 1], func=AF.Copy), r=[bkey], w=['idxf'])
            P.add('dve', lambda e: e.tensor_copy(out=idx_i[:], in_=idxf[:]), r=['idxf'], w=['idx_i'])
            if debug:
                P.add('sp', lambda e: e.dma_start(out=dbg['gidx'], in_=gidx_i[:]), r=['gidx_i'], w=['dbggidx'], dma=True)
                P.add('sp', lambda e: e.dma_start(out=dbg['wk'], in_=wk[:].rearrange("p t k -> p (t k)")), r=['wk'], w=['dbgwk'], dma=True)
                P.add('sp', lambda e: e.dma_start(out=dbg['idx'], in_=idx_i[:]), r=['idx_i'], w=['dbgidx'], dma=True)
            P.emit()

        with ExitStack() as es:
            P = Prog(nc, st)
            assert F == D
            KF = F // 128
            UW = min(512, D)
            CPU = UW // 128
            KHW = max(1, KD // 4)
            NKP = KD // KHW
            NR = 5
            NW = 4
            assert NR <= NBW
            wraw = [sb(es, "wraw%d" % i, [128, KHW, UW]) for i in range(NR)]
            wring = [sb(es, "wring%d" % i, [128, KHW, UW]) for i in range(NW)]
            NBB = 2
            bbuf = [sb(es, "bbuf%d" % i, [1, UW]) for i in range(NBB)]
            Xg = [sb(es, "Xg%d" % i, [128, D]) for i in range(NS)]
            XT = sb(es, "XT", [128, KD, CAP])
            actT = sb(es, "actT", [128, KF, CAP])
            eg = [sb(es, "eg%d" % i, [128, UW]) for i in range(2)]
            esg = [sb(es, "esg%d" % i, [128, UW]) for i in range(2)]
            eu = [sb(es, "eu%d" % i, [128, UW]) for i in range(2)]
            geg = [sb(es, "geg%d" % i, [128, UW]) for i in range(NS)]
            actp = [sb(es, "actp%d" % i, [128, UW]) for i in range(NS)]
            ystg = [sb(es, "ystg%d" % i, [128, UW]) for i in range(2)]
            assert 2 * NS + 2 <= 8
            banks = [(psb(es, "p6b%d" % i), ('p6b', i)) for i in range(2 * NS + 2)]
            sets = [banks[0:NS], banks[NS:2 * NS]]
            tbs = banks[2 * NS:2 * NS + 2]
            units = []
            for ee in range(E):
                for u in range(F // UW):
                    units.append((ee, 'g', u))
                    units.append((ee, 'u', u))
                for u in range(D // UW):
                    units.append((ee, 'd', u))
            NUE = 2 * (F // UW) + D // UW
            total = len(units) * NKP
            AR = min(3, NW - 1)
            AD = NR
            tcnt, bcnt, ycnt, ecnt, ucnt = [0], [0], [0], [0], [0]

            def issue_dma(i):
                ee, kind, u = units[i // NKP]
                p = i % NKP
                rows = slice(p * KHW * 128, (p + 1) * KHW * 128)
                if kind == 'g':
                    a = w_gu[ee, rows, u * UW:(u + 1) * UW]
                elif kind == 'u':
                    a = w_gu[ee, rows, F + u * UW:F + (u + 1) * UW]
                else:
                    a = w_dn[ee, rows, u * UW:(u + 1) * UW]
                src = a.rearrange("(k p) n -> p k n", p=128)
                buf = wraw[i % NR]
                P.add('sp', lambda e, buf=buf, src=src: e.dma_start(out=buf[:], in_=src), w=[('raw', i % NR)], dma=True, ring=i % NR)

            def issue_round(i):
                rw, rk = wraw[i % NR], ('raw', i % NR)
                buf, key = wring[i % NW], ('wr', i % NW)
                if i % 2 == 0:
                    P.add('dve', lambda e, buf=buf, rw=rw: e.tensor_copy(out=r32(buf[:]), in_=rw[:]), r=[rk], w=[key])
                else:
                    P.add('act', lambda e, buf=buf, rw=rw: e.activation(out=r32(buf[:]), in_=rw[:], func=AF.Copy), r=[rk], w=[key])

            def emit_gather(ee):
                lst = []
                for s_ in range(NS):
                    xg, xgk = Xg[s_], ('Xg', s_)
                    col = ee * NS + s_
                    P.add('pool', lambda e, xg=xg, col=col: e.indirect_dma_start(out=xg[:], out_offset=None, in_=H2, in_offset=bass.IndirectOffsetOnAxis(ap=idx_i[:, col:col + 1], axis=0)),
                          r=['idx_i'], w=[xgk], dma=True)
                    lst.append((xg, xgk))
                return lst

            def emit_xt(lst):
                for s_, (xg, xgk) in enumerate(lst):
                    for k4 in range(0, KD, 4):
                        bank, bkey = tbs[tcnt[0] % 2]
                        tcnt[0] += 1
                        nk = min(4, KD - k4)
                        for kk in range(nk):
                            P.add('pe', lambda e, k=k4 + kk, kk=kk, bank=bank, xg=xg: e.transpose(bank[:, kk * 128:(kk + 1) * 128], xg[:, k * 128:(k + 1) * 128], ident), r=[xgk, 'tabs'], w=[bkey])
                        P.add('act', lambda e, bank=bank, k4=k4, nk=nk, s_=s_: e.activation(out=r32(XT[:, k4:k4 + nk, s_ * 128:(s_ + 1) * 128]), in_=bank[:, 0:nk * 128].rearrange("p (k t) -> p k t", k=nk), func=AF.Copy),
                              r=[bkey], w=[('XT', s_)])

            def make_T(ap_, apk, u, s_):
                def emit_T():
                    tb, tbk = tbs[tcnt[0] % 2]
                    tcnt[0] += 1
                    for kk in range(CPU):
                        P.add('pe', lambda e, kk=kk, tb=tb: e.transpose(tb[:, kk * 128:(kk + 1) * 128], ap_[:, kk * 128:(kk + 1) * 128], ident), r=[apk, 'tabs'], w=[tbk])
                    P.add('act', lambda e, tb=tb: e.activation(out=r32(actT[:, u * CPU:(u + 1) * CPU, s_ * 128:(s_ + 1) * 128]),
                                                               in_=tb[:, 0:CPU * 128].rearrange("p (k t) -> p k t", k=CPU), func=AF.Copy),
                          r=[tbk], w=[('actT', s_, u)])
                return emit_T

            for i in range(min(NR, total)):
                issue_dma(i)
            for i in range(min(AR, total)):
                issue_round(i)
            for i in range(NR, min(AD, total)):
                issue_dma(i)
            emit_xt(emit_gather(0))
            pendT = []
            next_gl = None
            for ui, (ee, kind, u) in enumerate(units):
                bset = sets[ucnt[0] % 2]
                ucnt[0] += 1
                if kind == 'd' and u == 0 and ee + 1 < E:
                    next_gl = emit_gather(ee + 1)
                if kind == 'd' and u == 0 and (F // UW - 1) * CPU < KHW:
                    while pendT:
                        pendT.pop(0)()
                bb, bbk = bbuf[bcnt[0] % NBB], ('bbuf', bcnt[0] % NBB)
                bcnt[0] += 1
                if kind == 'g':
                    bsrc = b_gu[ee:ee + 1, u * UW:(u + 1) * UW]
                elif kind == 'u':
                    bsrc = b_gu[ee:ee + 1, F + u * UW:F + (u + 1) * UW]
                else:
                    bsrc = b_dn[ee:ee + 1, u * UW:(u + 1) * UW]
                P.add('pool', lambda e, bb=bb, bsrc=bsrc: e.dma_start(out=r32(bb[0:1, 0:UW]), in_=bsrc), w=[bbk], dma=True)
                for p in range(NKP):
                    i = ui * NKP + p
                    if i + AR < total:
                        issue_round(i + AR)
                    if i + AD < total:
                        issue_dma(i + AD)
                    wb, wbk = wring[i % NW], ('wr', i % NW)
                    for s_ in range(NS):
                        bank, bkey = bset[s_]
                        for kk in range(KHW):
                            k = p * KHW + kk
                            if kind == 'd':
                                lhs, rk = actT[:, k, s_ * 128:(s_ + 1) * 128], ('actT', s_, k // CPU)
                            else:
                                lhs, rk = XT[:, k, s_ * 128:(s_ + 1) * 128], ('XT', s_)
                            P.add('pe', lambda e, k=k, kk=kk, bank=bank, lhs=lhs, wb=wb: e.matmul(bank[:, 0:UW], r32(lhs), r32(wb[:, kk, :]), start=(k == 0), stop=False),
                                  r=[rk, wbk], w=[bkey])
                    if p == 0:
                        while pendT:
                            pendT.pop(0)()
                    if p == min(1, NKP - 1) and kind == 'd' and u == 0 and next_gl is not None:
                        emit_xt(next_gl)
                        next_gl = None
                for s_ in range(NS):
                    bank, bkey = bset[s_]
                    P.add('pe', lambda e, bank=bank, bb=bb: e.matmul(bank[:, 0:UW], r32(ones1r[0:1, :]), r32(bb[0:1, 0:UW]), start=False, stop=True), r=['ones1r', bbk], w=[bkey])
                for s_ in range(NS):
                    bank, bkey = bset[s_]
                    if kind == 'g':
                        i2 = ecnt[0] % 2
                        ecnt[0] += 1
                        P.add('dve', lambda e, bank=bank, i2=i2: e.tensor_scalar(out=eg[i2][:], in0=bank[:, 0:UW], scalar1=7.0, scalar2=None, op0=ALU.min), r=[bkey], w=[('eg', i2)])
                        P.add('act', lambda e, i2=i2: e.activation(out=esg[i2][:], in_=eg[i2][:], func=AF.Sigmoid, scale=1.702), r=[('eg', i2)], w=[('esg', i2)])
                        P.add('pool', lambda e, i2=i2, s_=s_: e.tensor_tensor(out=geg[s_][:], in0=eg[i2][:], in1=esg[i2][:], op=ALU.mult), r=[('eg', i2), ('esg', i2)], w=[('geg', s_)])
                    elif kind == 'u':
                        i2 = ecnt[0] % 2
                        ecnt[0] += 1
                        P.add('dve', lambda e, bank=bank, i2=i2: e.tensor_scalar(out=eu[i2][:], in0=bank[:, 0:UW], scalar1=-7.0, scalar2=7.0, op0=ALU.max, op1=ALU.min), r=[bkey], w=[('eu', i2)])
                        P.add('dve', lambda e, i2=i2, s_=s_: e.scalar_tensor_tensor(out=actp[s_][:], in0=eu[i2][:], scalar=1.0, in1=geg[s_][:], op0=ALU.add, op1=ALU.mult),
                              r=[('eu', i2), ('geg', s_)], w=[('actp', s_)])
                        pendT.append(make_T(actp[s_], ('actp', s_), u, s_))
                    else:
                        yb, ybk = ystg[ycnt[0] % 2], ('ystg', ycnt[0] % 2)
                        if ycnt[0] % 2 == 0:
                            P.add('act', lambda e, bank=bank, yb=yb: e.activation(out=yb[:, 0:UW], in_=bank[:, 0:UW], func=AF.Copy), r=[bkey], w=[ybk])
                        else:
                            P.add('dve', lambda e, bank=bank, yb=yb: e.tensor_copy(out=yb[:, 0:UW], in_=bank[:, 0:UW]), r=[bkey], w=[ybk])
                        ycnt[0] += 1
                        r0 = (ee * NS + s_) * 128
                        P.add('pool', lambda e, yb=yb, r0=r0, u=u: e.dma_start(out=YS[r0:r0 + 128, u * UW:(u + 1) * UW], in_=yb[:, 0:UW]), r=[ybk], w=[('YS', ee, s_, u)], dma=True)
            assert not pendT
            if debug:
                P.add('sp', lambda e: e.dma_start(out=dbg['ys'], in_=YS), r=[('YS', ee, s_, u) for ee in range(E) for s_ in range(NS) for u in range(D // UW)], w=['dbgys'], dma=True)
            P.emit()

        with ExitStack() as es:
            P = Prog(nc, st)
            Yg = [sb(es, "Yg%d" % i, [128, D]) for i in range(4)]
            acc = [sb(es, "acc%d" % i, [128, D]) for i in range(2)]
            x1t = [sb(es, "x1t%d" % i, [128, D]) for i in range(2)]
            g2t = sb(es, "g2t", [128, D])
            fgb = sb(es, "fgb", [128, D])
            ss7 = [sb(es, "ss7%d" % i, [128, 4]) for i in range(2)]
            P.add('act', lambda e: e.dma_start(out=fgb[:], in_=final_g[0, :].partition_broadcast(128)), w=['fgb'], dma=True)
            cur_ty = -1
            gc = 0
            for t in range(NT):
                ty = 0 if t < TP else 1
                if ty != cur_ty:
                    load_mod(P, g2t, 'g2t', 5, ty)
                    cur_ty = ty
                i2 = t % 2
                ac, ack = acc[i2], ('acc', i2)
                xt, xtk = x1t[i2], ('x1t', i2)
                ss, sk = ss7[i2], ('ss7', i2)
                P.add('sp', lambda e, t=t, xt=xt: e.dma_start(out=xt[:], in_=X1[t * 128:(t + 1) * 128, :]), w=[xtk], dma=True)
                for k in range(TOPK):
                    yg, ygk = Yg[gc % 4], ('Yg', gc % 4)
                    gc += 1
                    c_ = t * TOPK + k
                    P.add('pool', lambda e, yg=yg, c_=c_: e.indirect_dma_start(out=yg[:], out_offset=None, in_=YS, in_offset=bass.IndirectOffsetOnAxis(ap=gidx_i[:, c_:c_ + 1], axis=0)),
                          r=['gidx_i'], w=[ygk], dma=True)
                    if k == 0:
                        P.add('dve', lambda e, yg=yg, ac=ac, t=t, k=k: e.tensor_scalar(out=ac[:], in0=yg[:], scalar1=wk[:, t, k:k + 1], scalar2=None, op0=ALU.mult), r=[ygk, 'wk'], w=[ack])
                    else:
                        P.add('dve', lambda e, yg=yg, ac=ac, t=t, k=k: e.scalar_tensor_tensor(out=ac[:], in0=yg[:], scalar=wk[:, t, k:k + 1], in1=ac[:], op0=ALU.mult, op1=ALU.add), r=[ygk, 'wk', ack], w=[ack])
                P.add('pool', lambda e, ac=ac: e.tensor_tensor(out=ac[:], in0=ac[:], in1=g2t[:], op=ALU.mult), r=[ack, 'g2t'], w=[ack])
                P.add('dve', lambda e, ac=ac, xt=xt: e.tensor_tensor(out=xt[:], in0=xt[:], in1=ac[:], op=ALU.add), r=[ack, xtk], w=[xtk])
                P.add('act', lambda e, ac=ac, xt=xt, ss=ss: e.activation(out=ac[:], in_=xt[:], func=AF.Square, accum_out=ss[:, 0:1]), r=[xtk, ack], w=[ack, sk])
                P.add('dve', lambda e, ss=ss: e.tensor_scalar(out=ss[:, 1:2], in0=ss[:, 0:1], scalar1=1.0 / D, scalar2=NORM_EPS, op0=ALU.mult, op1=ALU.add), r=[sk], w=[sk])
                P.add('act', lambda e, ss=ss: e.sqrt(ss[:, 3:4], ss[:, 1:2]), r=[sk], w=[sk])
                P.add('dve', lambda e, ss=ss: e.reciprocal(out=ss[:, 2:3], in_=ss[:, 3:4]), r=[sk], w=[sk])
                P.add('dve', lambda e, ac=ac, xt=xt, ss=ss: e.scalar_tensor_tensor(out=ac[:], in0=xt[:], scalar=ss[:, 2:3], in1=fgb[:], op0=ALU.mult, op1=ALU.mult), r=[xtk, sk, 'fgb', ack], w=[ack])
                P.add('sp', lambda e, t=t, ac=ac: e.dma_start(out=y[t * 128:(t + 1) * 128, :], in_=ac[:]), r=[ack], w=[('y', t)], dma=True)
            P.emit(final=True)
    return nc


def run_cfg(cfg, inputs, debug=False):
    D, TP, NT, H, CW = cfg.D, cfg.TP, cfg.NT, cfg.H, cfg.CW
    f = lambda a: np.ascontiguousarray(np.asarray(a, dtype=np.float32))
    x_prompt, x_sample = f(inputs['x_prompt']), f(inputs['x_sample'])
    state_ret, state_conv = f(inputs['state_ret'])[0], f(inputs['state_conv'])[0]
    c_prompt, c_sample = f(inputs['c_prompt']), f(inputs['c_sample'])
    B = x_prompt.shape[0]
    HALF = TP * 128
    shared = {
        'w_ada': f(inputs['w_ada'])[0], 'b_ada': f(inputs['b_ada']).reshape(1, -1), 'norm1_g': f(inputs['norm1_g']).reshape(1, -1),
        'norm2_g': f(inputs['norm2_g']).reshape(1, -1), 'w_in': f(inputs['w_in'])[0], 'conv_w': f(inputs['conv_w']).reshape(1, -1),
        'ret_gn': f(inputs['ret_gn']).reshape(1, -1), 'w_o': f(inputs['w_o'])[0], 'w_router': f(inputs['w_router'])[0],
        'b_router': f(inputs['b_router']).reshape(1, -1), 'w_gu': f(inputs['w_gu'])[0], 'b_gu': f(inputs['b_gu'])[0],
        'w_dn': f(inputs['w_dn'])[0], 'b_dn': f(inputs['b_dn'])[0], 'final_g': f(inputs['final_g']).reshape(1, -1),
    }
    in_maps = []
    for c in range(NCORES):
        b, half = c // 2, c % 2
        s0 = c * SEQ_PER_CORE
        xm = np.concatenate([x_prompt[b, half * HALF:(half + 1) * HALF], x_sample[s0:s0 + SEQ_PER_CORE].reshape(128, D)], axis=0)
        c32 = np.zeros((32, D), np.float32)
        c32[0] = c_prompt[b]
        c32[1:17] = c_sample[s0:s0 + SEQ_PER_CORE]
        m = dict(shared)
        m.update({'xm': xm, 'xp': np.ascontiguousarray(x_prompt[b, 0:HALF]), 'c32': c32,
                  'sret': np.ascontiguousarray(state_ret[s0:s0 + SEQ_PER_CORE]),
                  'sconv': np.ascontiguousarray(state_conv[s0:s0 + SEQ_PER_CORE].reshape(32, CW)),
                  'tab': make_tables(cfg, half)})
        in_maps.append(m)
    nc = build_program(cfg, debug=debug)
    res = run_bass_kernel_spmd(nc, in_maps, core_ids=list(range(NCORES)))
    R = res.results
    y_prompt = np.zeros((B, 2 * HALF, D), np.float32)
    y_sample = np.zeros((NCORES * SEQ_PER_CORE, DEC_SEQ, D), np.float32)
    ret_p = np.zeros((1, B, H, 128, 128), np.float32)
    conv_p = np.zeros((1, B, 2, CW), np.float32)
    ret_s = np.zeros((1, NCORES * SEQ_PER_CORE, H, 128, 128), np.float32)
    conv_s = np.zeros((1, NCORES * SEQ_PER_CORE, 2, CW), np.float32)
    for c in range(NCORES):
        b, half = c // 2, c % 2
        s0 = c * SEQ_PER_CORE
        yy = R[c]['y']
        y_prompt[b, half * HALF:(half + 1) * HALF] = yy[0:HALF]
        y_sample[s0:s0 + SEQ_PER_CORE] = yy[HALF:].reshape(SEQ_PER_CORE, DEC_SEQ, D)
        if half == 1:
            ret_p[0, b] = R[c]['ret_p']
            conv_p[0, b] = R[c]['conv_p']
        ret_s[0, s0:s0 + SEQ_PER_CORE] = R[c]['ret_s']
        conv_s[0, s0:s0 + SEQ_PER_CORE] = R[c]['conv_s'].reshape(SEQ_PER_CORE, DEC_SEQ, CW)[:, DEC_SEQ - 2:DEC_SEQ, :]
    outs = (y_prompt, y_sample, ret_p, conv_p, ret_s, conv_s)
    if debug:
        return outs, R
    return outs


def kernel(**inputs):
    cfg = Cfg(D=2048, E=32, TP=8)
    return run_cfg(cfg, inputs)
```
